# Optimizing a Trainium2 kernel written in Bass

```python
import math
import jax, jax.numpy as jnp
from jax import lax
import numpy as np

D_MODEL = 1024
BATCH = 8
SEQ = 2048
DEPTH = 2

CONV_CH = D_MODEL // 4
CONV_WIDTH = 31
MOBA_HEADS = 4
MOBA_HEAD_DIM = D_MODEL // 16
MOBA_WIDTH = MOBA_HEADS * MOBA_HEAD_DIM
MOBA_BLOCK = 256
MOBA_TOPK = 3
MOBA_Q_CHUNK = 64
GLA_HEADS = 4
GLA_WIDTH = D_MODEL // 2
GLA_DV = GLA_WIDTH // GLA_HEADS
GLA_DK = GLA_DV // 2
GLA_GATE_RANK = 16
GLA_TAU = 16.0
GLA_CHUNK = 32
MIX_WIDTH = CONV_CH + MOBA_WIDTH + GLA_WIDTH
D_FF = 2816
FFN_CONV_WIDTH = 3
NORM_EPS = 1e-6
NEG_INF = -1e30

COL_A = 2 * CONV_CH
COL_B = 3 * MOBA_WIDTH
COL_C_Q = GLA_HEADS * GLA_DK
COL_C_K = GLA_HEADS * GLA_DK
COL_C_V = GLA_WIDTH
COL_C_G = GLA_GATE_RANK
COL_C_R = GLA_WIDTH
COL_C = COL_C_Q + COL_C_K + COL_C_V + COL_C_G + COL_C_R
IN_COLS = COL_A + COL_B + COL_C

kernel_name = "hybrid_conformer_moba_gla_convffn"


def rms_norm(x, g, eps=NORM_EPS):
    xf = x.astype(jnp.float32)
    y = xf * lax.rsqrt(jnp.mean(xf * xf, axis=-1, keepdims=True) + eps)
    return (y * g.astype(jnp.float32)).astype(x.dtype)


def head_rms_norm(x, g, n_heads):
    B, S, W = x.shape
    xf = x.astype(jnp.float32).reshape(B, S, n_heads, W // n_heads)
    y = xf * lax.rsqrt(jnp.mean(xf * xf, axis=-1, keepdims=True) + NORM_EPS)
    return (y.reshape(B, S, W) * g.astype(jnp.float32)).astype(x.dtype)


def layer_norm(x, g, b, eps=1e-5):
    xf = x.astype(jnp.float32)
    mu = jnp.mean(xf, axis=-1, keepdims=True)
    var = jnp.mean(jnp.square(xf - mu), axis=-1, keepdims=True)
    y = (xf - mu) * lax.rsqrt(var + eps)
    return (y * g.astype(jnp.float32) + b.astype(jnp.float32)).astype(x.dtype)


def causal_depthwise_conv(x, w, b):
    W = w.shape[0]
    y = lax.conv_general_dilated(
        x, w[:, None, :].astype(x.dtype), window_strides=(1,), padding=[(W - 1, 0)],
        dimension_numbers=("NWC", "WIO", "NWC"), feature_group_count=x.shape[-1])
    return y + b.astype(x.dtype)


def conformer_conv(a_val, a_gate, conv_w, conv_b, ln_g, ln_b):
    h = a_val * jax.nn.sigmoid(a_gate)
    h = causal_depthwise_conv(h, conv_w, conv_b)
    h = layer_norm(h, ln_g, ln_b)
    return jax.nn.silu(h)


def moba_attention(q, k, v):
    B, S, H, dh = q.shape
    s_pad = ((S + MOBA_BLOCK - 1) // MOBA_BLOCK) * MOBA_BLOCK
    pad = [(0, 0), (0, s_pad - S), (0, 0), (0, 0)]
    qp = jnp.pad(q, pad).transpose(0, 2, 1, 3)
    kp = jnp.pad(k, pad).transpose(0, 2, 1, 3)
    vp = jnp.pad(v, pad).transpose(0, 2, 1, 3)
    nb = s_pad // MOBA_BLOCK
    topk = min(MOBA_TOPK, nb)
    kb = kp.reshape(B, H, nb, MOBA_BLOCK, dh)
    vb = vp.reshape(B, H, nb, MOBA_BLOCK, dh)
    k_mean = jnp.mean(kb.astype(jnp.float32), axis=3)
    nq = s_pad // MOBA_Q_CHUNK
    q_chunks = qp.reshape(B, H, nq, MOBA_Q_CHUNK, dh).transpose(2, 0, 1, 3, 4)
    scale = 1.0 / math.sqrt(dh)
    chunks_per_block = MOBA_BLOCK // MOBA_Q_CHUNK
    gather_blocks = jax.vmap(jax.vmap(lambda t, i: t[i]))

    def one_chunk(args):
        c, qc = args
        own = c // chunks_per_block
        q_pos = c * MOBA_Q_CHUNK + jnp.arange(MOBA_Q_CHUNK)
        gate = jnp.einsum("bhqd,bhnd->bhqn", qc.astype(jnp.float32), k_mean)
        gate = jnp.where(jnp.arange(nb) < own, gate, NEG_INF)
        _, idx = lax.top_k(gate, topk)
        valid = idx < own
        ks = gather_blocks(kb, idx)
        vs = gather_blocks(vb, idx)
        l_sel = jnp.einsum("bhqd,bhqtkd->bhqtk", qc, ks).astype(jnp.float32) * scale
        l_sel = jnp.where(valid[..., None], l_sel, NEG_INF).reshape(B, H, MOBA_Q_CHUNK, topk * MOBA_BLOCK)
        k_own = lax.dynamic_index_in_dim(kb, own, axis=2, keepdims=False)
        v_own = lax.dynamic_index_in_dim(vb, own, axis=2, keepdims=False)
        k_pos = own * MOBA_BLOCK + jnp.arange(MOBA_BLOCK)
        l_own = jnp.einsum("bhqd,bhkd->bhqk", qc, k_own).astype(jnp.float32) * scale
        l_own = jnp.where(k_pos[None, :] <= q_pos[:, None], l_own, NEG_INF)
        p = jax.nn.softmax(jnp.concatenate([l_sel, l_own], axis=-1), axis=-1)
        p_sel = p[..., : topk * MOBA_BLOCK].reshape(B, H, MOBA_Q_CHUNK, topk, MOBA_BLOCK).astype(vs.dtype)
        p_own = p[..., topk * MOBA_BLOCK:].astype(v_own.dtype)
        return (jnp.einsum("bhqtk,bhqtkd->bhqd", p_sel, vs)
                + jnp.einsum("bhqk,bhkd->bhqd", p_own, v_own))

    out = lax.map(one_chunk, (jnp.arange(nq), q_chunks))
    out = out.transpose(1, 0, 3, 2, 4).reshape(B, s_pad, H, dh)
    return out[:, :S].astype(q.dtype)


def gla_chunked(q, k, v, log_a):
    B, S, H, DK = q.shape
    DV = v.shape[-1]
    C = GLA_CHUNK
    nc = S // C

    def to_chunks(t):
        return t.astype(jnp.float32).reshape(B, nc, C, H, t.shape[-1]).transpose(0, 3, 1, 2, 4)

    qc, kc, vc, gc = to_chunks(q), to_chunks(k), to_chunks(v), to_chunks(log_a)
    G = jnp.cumsum(gc, axis=3)
    gamma = G[:, :, :, -1:, :]
    qg = qc * jnp.exp(G)
    kg = kc * jnp.exp(-G)
    kd = kc * jnp.exp(gamma - G)
    tril = jnp.tril(jnp.ones((C, C), dtype=bool))
    attn = jnp.where(tril, jnp.einsum("bhnid,bhnjd->bhnij", qg, kg), 0.0)
    o_intra = jnp.einsum("bhnij,bhnje->bhnie", attn, vc)
    d_state = jnp.einsum("bhnjd,bhnje->bhnde", kd, vc)
    decay = jnp.exp(gamma[:, :, :, 0, :])

    def step(state, inp):
        d, ds = inp
        return d[..., None] * state + ds, state

    _, s_prev = lax.scan(step, jnp.zeros((B, H, DK, DV), jnp.float32),
                         (jnp.moveaxis(decay, 2, 0), jnp.moveaxis(d_state, 2, 0)))
    s_prev = jnp.moveaxis(s_prev, 0, 2)
    o = o_intra + jnp.einsum("bhnid,bhnde->bhnie", qg, s_prev)
    return o.transpose(0, 2, 3, 1, 4).reshape(B, S, H, DV).astype(v.dtype)


def setup_inputs(seed: int = 0) -> dict:
    key = jax.random.key(seed)
    ks = jax.random.split(key, 20)
    f32 = jnp.float32
    n = lambda k, shape, s: jax.random.normal(k, shape, f32) * s
    return {
        "x": n(ks[0], (BATCH, SEQ, D_MODEL), 1.0),
        "norm_mix_g": 1.0 + n(ks[1], (DEPTH, D_MODEL), 0.05),
        "w_in": n(ks[2], (DEPTH, D_MODEL, IN_COLS), D_MODEL ** -0.5),
        "conv_w": n(ks[3], (DEPTH, CONV_WIDTH, CONV_CH), CONV_WIDTH ** -0.5),
        "conv_b": n(ks[4], (DEPTH, CONV_CH), 0.02),
        "conv_ln_g": 1.0 + n(ks[5], (DEPTH, CONV_CH), 0.05),
        "conv_ln_b": n(ks[6], (DEPTH, CONV_CH), 0.02),
        "moba_out_g": 1.0 + n(ks[7], (DEPTH, MOBA_WIDTH), 0.05),
        "gla_gate_w": n(ks[8], (DEPTH, GLA_GATE_RANK, GLA_HEADS * GLA_DK), GLA_GATE_RANK ** -0.5),
        "gla_gate_b": n(ks[9], (DEPTH, GLA_HEADS * GLA_DK), 0.1),
        "gla_out_g": 1.0 + n(ks[10], (DEPTH, GLA_WIDTH), 0.05),
        "w_out": n(ks[11], (DEPTH, MIX_WIDTH, D_MODEL), MIX_WIDTH ** -0.5),
        "norm_ffn_g": 1.0 + n(ks[12], (DEPTH, D_MODEL), 0.05),
        "ffn_w_up": n(ks[13], (DEPTH, D_MODEL, 2 * D_FF), D_MODEL ** -0.5),
        "ffn_conv_w": n(ks[14], (DEPTH, FFN_CONV_WIDTH, 2 * D_FF), FFN_CONV_WIDTH ** -0.5),
        "ffn_conv_b": n(ks[15], (DEPTH, 2 * D_FF), 0.02),
        "ffn_w_down": n(ks[16], (DEPTH, D_FF, D_MODEL), D_FF ** -0.5),
        "final_g": 1.0 + n(ks[17], (D_MODEL,), 0.05),
    }


def reference(x, norm_mix_g, w_in, conv_w, conv_b, conv_ln_g, conv_ln_b, moba_out_g,
              gla_gate_w, gla_gate_b, gla_out_g, w_out, norm_ffn_g, ffn_w_up, ffn_conv_w,
              ffn_conv_b, ffn_w_down, final_g):
    B, S, _ = x.shape
    o1 = COL_A
    o2 = o1 + COL_B
    o3 = o2 + COL_C_Q
    o4 = o3 + COL_C_K
    o5 = o4 + COL_C_V
    o6 = o5 + COL_C_G
    for l in range(DEPTH):
        h = rms_norm(x, norm_mix_g[l])
        p = h @ w_in[l].astype(h.dtype)
        y_a = conformer_conv(p[..., :CONV_CH], p[..., CONV_CH:o1],
                             conv_w[l], conv_b[l], conv_ln_g[l], conv_ln_b[l])
        qkv = p[..., o1:o2].reshape(B, S, 3, MOBA_HEADS, MOBA_HEAD_DIM)
        y_b = moba_attention(qkv[:, :, 0], qkv[:, :, 1], qkv[:, :, 2]).reshape(B, S, MOBA_WIDTH)
        y_b = head_rms_norm(y_b, moba_out_g[l], MOBA_HEADS)
        q_c = p[..., o2:o3].reshape(B, S, GLA_HEADS, GLA_DK) * (GLA_DK ** -0.5)
        k_c = p[..., o3:o4].reshape(B, S, GLA_HEADS, GLA_DK)
        v_c = p[..., o4:o5].reshape(B, S, GLA_HEADS, GLA_DV)
        gate_logit = (p[..., o5:o6] @ gla_gate_w[l].astype(p.dtype)).astype(jnp.float32) \
            + gla_gate_b[l].astype(jnp.float32)
        log_a = (jax.nn.log_sigmoid(gate_logit) / GLA_TAU).reshape(B, S, GLA_HEADS, GLA_DK)
        y_c = gla_chunked(q_c, k_c, v_c, log_a).reshape(B, S, GLA_WIDTH)
        y_c = head_rms_norm(y_c, gla_out_g[l], GLA_HEADS) * jax.nn.silu(p[..., o6:])
        y = jnp.concatenate([y_a, y_b, y_c], axis=-1) @ w_out[l].astype(x.dtype)
        x = x + y
        h = rms_norm(x, norm_ffn_g[l])
        u = causal_depthwise_conv(h @ ffn_w_up[l].astype(h.dtype), ffn_conv_w[l], ffn_conv_b[l])
        x = x + (jax.nn.silu(u[..., :D_FF]) * u[..., D_FF:]) @ ffn_w_down[l].astype(x.dtype)
    return rms_norm(x, final_g)
```

```python
import numpy as np
from contextlib import ExitStack
import concourse.bass as bass
import concourse.mybir as mybir
from concourse.bass_utils import run_bass_kernel_spmd

F32 = mybir.dt.float32
BF16 = mybir.dt.bfloat16
AF = mybir.ActivationFunctionType
ALU = mybir.AluOpType
AX = mybir.AxisListType

ENGS = ("pe", "act", "dve", "pool", "sp")
S = 2048
D = 1024
L = 2
NPAR = 276
BIG = 30000.0
N_CORES = 8


class Res:
    __slots__ = ("name", "w", "readers", "dsem", "dcnt", "excl")

    def __init__(self, name, excl=False):
        self.name = name
        self.excl = excl
        self.w = None
        self.readers = []
        self.dsem = None
        self.dcnt = 0

    def inherit(self, *others):
        for o in others:
            if o.w is not None:
                self.readers.append(o.w)
            self.readers.extend(o.readers)
        return self


class Op:
    __slots__ = ("id", "eng", "fn", "deps", "dur", "dma", "sem", "semval", "idx", "tag")


class Prog:
    def __init__(self, nc, stack):
        self.nc = nc
        self.stack = stack
        self.ops = []
        self.sem = {e: stack.enter_context(nc.semaphore("c_" + e)) for e in ENGS}
        self.nsem = 0
        self.out_ops = {}

    def new_dsem(self):
        self.nsem += 1
        return self.stack.enter_context(self.nc.semaphore("d%d" % self.nsem))

    def _mk(self, e, fn, deps, dur):
        o = Op()
        o.id = len(self.ops)
        o.eng = e
        o.fn = fn
        o.deps = deps
        o.dur = dur
        o.dma = False
        o.sem = None
        o.semval = 0
        o.idx = 0
        o.tag = getattr(self, 'tag', '')
        self.ops.append(o)
        return o

    def op(self, e, fn, reads=(), writes=(), dur=300.0):
        if any(r.excl for r in reads):
            writes = list(writes) + [r for r in reads if r.excl and r not in writes]
            reads = [r for r in reads if not r.excl]
        deps = set()
        for r in reads:
            if r.w is not None:
                deps.add(r.w)
        for w in writes:
            if w.w is not None:
                deps.add(w.w)
            deps.update(w.readers)
        o = self._mk(e, fn, deps, dur)
        for r in reads:
            r.readers.append(o.id)
        for w in writes:
            w.w = o.id
            w.readers = []
        return o.id

    def dma(self, q, out, in_, reads=(), writes=(), nbytes=1 << 20):
        deps = set()
        for r in reads:
            if r.w is not None:
                deps.add(r.w)
        for w in writes:
            if w.w is not None:
                deps.add(w.w)
            deps.update(w.readers)
        tgt = writes[0] if writes else reads[0]
        if tgt.dsem is None:
            tgt.dsem = self.new_dsem()
        tgt.dcnt += 16
        o = self._mk(q, lambda eng: eng.dma_start(out=out, in_=in_), deps, 2000.0 + nbytes / 200.0)
        o.dma = True
        o.sem = tgt.dsem
        o.semval = tgt.dcnt
        if not writes:
            self.out_ops[id(tgt.dsem)] = o.id
        for r in reads:
            r.readers.append(o.id)
        for w in writes:
            w.w = o.id
            w.readers = []
        return o.id

    def schedule(self):
        import heapq
        ops = self.ops
        n = len(ops)
        succ = [[] for _ in range(n)]
        indeg = [0] * n
        for o in ops:
            o.deps.discard(o.id)
            indeg[o.id] = len(o.deps)
            for d in o.deps:
                succ[d].append(o.id)
        finish = [0.0] * n
        ready_t = [0.0] * n
        fut = {e: [] for e in ENGS}
        avail = {e: [] for e in ENGS}
        free = {e: 0.0 for e in ENGS}
        for o in ops:
            if indeg[o.id] == 0:
                heapq.heappush(fut[o.eng], (0.0, o.id))
        order = []
        per_eng = {e: [] for e in ENGS}
        done = 0
        while done < n:
            best = None
            for e in ENGS:
                f, a = fut[e], avail[e]
                while f and f[0][0] <= free[e]:
                    heapq.heappush(a, heapq.heappop(f)[1])
                if a:
                    cand = (free[e], a[0], e, True)
                elif f:
                    cand = (f[0][0], f[0][1], e, False)
                else:
                    continue
                if best is None or cand[:2] < best[:2]:
                    best = cand
            st, oid, e, from_avail = best
            if from_avail:
                heapq.heappop(avail[e])
            else:
                heapq.heappop(fut[e])
            o = ops[oid]
            if o.dma:
                free[e] = st + 60.0
                finish[oid] = st + o.dur
            else:
                free[e] = st + o.dur
                finish[oid] = st + o.dur
            per_eng[e].append(oid)
            o.idx = len(per_eng[e])
            order.append(oid)
            done += 1
            for s_ in succ[oid]:
                so = ops[s_]
                lat = 0.0 if (so.eng == e and e == "pe" and not o.dma) else 120.0
                t = finish[oid] + lat
                if t > ready_t[s_]:
                    ready_t[s_] = t
                indeg[s_] -= 1
                if indeg[s_] == 0:
                    heapq.heappush(fut[so.eng], (ready_t[s_], s_))
        self.est_ns = max(finish) if finish else 0.0
        return order

    def finish(self, e="sp"):
        deps = set(self.out_ops.values())
        o = self._mk(e, None, deps, 10.0)
        return o.id

    def emit(self):
        nc = self.nc
        ops = self.ops
        order = self.schedule()
        cnt = {e: 0 for e in ENGS}
        for oid in order:
            o = ops[oid]
            if not o.dma and o.fn is not None:
                cnt[o.eng] += 1
                o.idx = cnt[o.eng]
        streams = {e: [] for e in ENGS}
        seen = {e: {f: 0 for f in ENGS} for e in ENGS}
        seen_d = {e: {} for e in ENGS}
        snaps = {}
        for oid in order:
            o = ops[oid]
            e = o.eng
            for d in sorted(o.deps):
                do = ops[d]
                if do.dma:
                    k = id(do.sem)
                    if seen_d[e].get(k, 0) >= do.semval:
                        continue
                    streams[e].append(("wait", do.sem, do.semval))
                    seen_d[e][k] = do.semval
                else:
                    f = do.eng
                    if f == e and e == "pe":
                        continue
                    if seen[e][f] >= do.idx:
                        continue
                    streams[e].append(("wait", self.sem[f], do.idx))
                    seen[e][f] = do.idx
                    sn = snaps[d]
                    for g, v in sn[0].items():
                        if g != e and seen[e][g] < v:
                            seen[e][g] = v
                    for k, v in sn[1].items():
                        if seen_d[e].get(k, 0) < v:
                            seen_d[e][k] = v
            if o.fn is None:
                continue
            if o.dma:
                streams[e].append(("op", o.fn, o.sem, 16))
            else:
                snaps[oid] = (dict(seen[e]), dict(seen_d[e]))
                streams[e].append(("op", o.fn, self.sem[e], 1))
        self.nwaits = sum(1 for e in ENGS for it in streams[e] if it[0] == "wait")

        def run(eng, items):
            for it in items:
                if it[0] == "wait":
                    eng.wait_ge(it[1], it[2])
                else:
                    it[1](eng).then_inc(it[2], it[3])

        with nc.Block() as block:
            @block.tensor
            def _(pe):
                run(pe, streams["pe"])

            @block.scalar
            def _(act):
                run(act, streams["act"])

            @block.vector
            def _(dve):
                run(dve, streams["dve"])

            @block.gpsimd
            def _(pool):
                run(pool, streams["pool"])

            @block.sync
            def _(sp):
                run(sp, streams["sp"])


class Arena:
    def __init__(self, nc, nbytes):
        self.t = nc.alloc_sbuf_tensor("arena", [128, nbytes // 2], BF16)
        self.nbytes = nbytes
        self.live = []

    def alloc(self, name, off, shape, dtype, nres=1):
        esz = 4 if dtype == F32 else 2
        n = int(np.prod(shape[1:]))
        nb = n * esz
        assert off % 32 == 0 and off + nb <= self.nbytes, (name, off, nb, self.nbytes)
        ap = self.t[0:shape[0], off // 2: (off + nb) // 2]
        if dtype != BF16:
            ap = ap.bitcast(dtype)
        if len(shape) > 2:
            names = " ".join("d%d" % i for i in range(1, len(shape)))
            kw = {"d%d" % i: shape[i] for i in range(1, len(shape))}
            ap = ap.rearrange("p (%s) -> p %s" % (names, names), **kw)
        res = [Res("%s%d" % (name, i)) for i in range(nres)]
        keep = []
        for (o0, o1, rl) in self.live:
            if o0 < off + nb and off < o1:
                for r in res:
                    r.inherit(*rl)
                if o0 < off:
                    keep.append((o0, off, rl))
                if off + nb < o1:
                    keep.append((off + nb, o1, rl))
            else:
                keep.append((o0, o1, rl))
        keep.append((off, off + nb, res))
        self.live = keep
        return ap, (res[0] if nres == 1 else res)


XT_OFF = 0
HT_OFF = 65536
YG_OFF = HT_OFF + 32800
CF_OFF = YG_OFF + 32768
NCF = 128 * 5 + 512
CB_OFF = CF_OFF + NCF * 4
NCB = 1024
CE_OFF = CB_OFF + NCB * 2
PR_OFF = CE_OFF
GW_OFF = PR_OFF + L * NPAR * 4
SCR = GW_OFF + 2 * 256 * 4
SCR_SZ = 69 * 1024
ARENA_BYTES = SCR + SCR_SZ

FFN_TILES = [(0, 410), (410, 820), (820, 1230), (1230, 1640), (1640, 2048)]
ROUNDS = [list(range(0, 8)), list(range(8, 15)), list(range(15, 22))]


class StopBuild(Exception):
    pass


def build_program(stage=0, cstop=0):
    nc = bass.Bass("TRN2", target_bir_lowering=False)
    d_x = nc.dram_tensor("xT", [8, 128, S], F32, kind="ExternalInput").ap()
    d_win = nc.dram_tensor("w_in", [L, 128, 8, 2832], F32, kind="ExternalInput").ap()
    d_wout = nc.dram_tensor("w_out", [L, 128, 8, 1024], F32, kind="ExternalInput").ap()
    d_wup = nc.dram_tensor("w_up", [L, 22, 128, 8, 256], F32, kind="ExternalInput").ap()
    d_wdn = nc.dram_tensor("w_dn", [L, 22, 128, 1024], F32, kind="ExternalInput").ap()
    d_par = nc.dram_tensor("params", [128, L, NPAR], F32, kind="ExternalInput").ap()
    d_gw = nc.dram_tensor("gatew", [17, L, 256], F32, kind="ExternalInput").ap()
    d_cf = nc.dram_tensor("constF", [128, NCF], F32, kind="ExternalInput").ap()
    d_cb = nc.dram_tensor("constB", [128, NCB], F32, kind="ExternalInput").ap()
    d_e8 = nc.dram_tensor("constE8", [8, S], F32, kind="ExternalInput").ap()
    d_out = nc.dram_tensor("outT", [8, 128, S], F32, kind="ExternalOutput").ap()

    with ExitStack() as st:
        P = Prog(nc, st)
        A = Arena(nc, ARENA_BYTES)
        PS = [st.enter_context(nc.psum_tensor("ps%d" % i, [128, 512], F32)) for i in range(8)]
        PSR = [Res("psb%d" % i, excl=True) for i in range(8)]

        def fsz(ap):
            n = 1
            for d in ap.shape[1:]:
                n *= d
            return n

        def vdur(eng, n, accel=1.0):
            if eng == "pool":
                return n * 2.3 + 100.0
            return n / (0.96 * accel) + 130.0

        def MM(out, lhsT, rhs, start, stop, reads, writes):
            mult = 4.0 if lhsT.dtype == F32 else 1.0
            P.op("pe", lambda e: e.matmul(out, lhsT, rhs, start=start, stop=stop), reads, writes,
                 dur=max(fsz(out), 64) / 2.4 * mult + 12.0)

        def TR(out, in_, ident, reads, writes):
            P.op("pe", lambda e: e.transpose(out, in_, ident), reads, writes, dur=260.0)

        def ACT(out, in_, func, reads, writes, bias=None, scale=None):
            kw = {}
            if bias is not None:
                kw["bias"] = bias
            if scale is not None:
                kw["scale"] = scale
            P.op("act", lambda e: e.activation(out=out, in_=in_, func=func, **kw), reads, writes,
                 dur=(fsz(out) + 220.0) / 1.2)

        def TT(eng, out, in0, in1, op, reads, writes):
            P.op(eng, lambda e: e.tensor_tensor(out=out, in0=in0, in1=in1, op=op), reads, writes, dur=vdur(eng, fsz(out)))

        def TS(eng, out, in0, s1, op0, reads, writes, s2=None, op1=None):
            if op1 is None:
                P.op(eng, lambda e: e.tensor_scalar(out=out, in0=in0, scalar1=s1, scalar2=None, op0=op0), reads, writes,
                     dur=vdur(eng, fsz(out)))
            else:
                P.op(eng, lambda e: e.tensor_scalar(out=out, in0=in0, scalar1=s1, scalar2=s2, op0=op0, op1=op1),
                     reads, writes, dur=vdur(eng, fsz(out)))

        def STT(eng, out, in0, scalar, in1, op0, op1, reads, writes):
            P.op(eng, lambda e: e.scalar_tensor_tensor(out=out, in0=in0, scalar=scalar, in1=in1, op0=op0, op1=op1),
                 reads, writes, dur=vdur(eng, fsz(out)))

        def CP(eng, out, in_, reads, writes):
            P.op(eng, lambda e: e.tensor_copy(out=out, in_=in_), reads, writes, dur=vdur(eng, fsz(out), 2.0))

        def RSUM(eng, out, in_, reads, writes):
            P.op(eng, lambda e: e.tensor_reduce(out=out, in_=in_, axis=AX.X, op=ALU.add), reads, writes,
                 dur=vdur(eng, fsz(in_)))

        def MSET(eng, ap, val, writes):
            P.op(eng, lambda e: e.memset(ap, val), (), writes, dur=vdur(eng, fsz(ap), 2.0))

        def DMA(q, out, in_, reads=(), writes=()):
            P.dma(q, out, in_, reads=reads, writes=writes, nbytes=out.shape[0] * fsz(out) * 4)

        def tiles_of(t0, t1):
            return list(range(t0 // 512, (t1 - 1) // 512 + 1))

        XT, XR = A.alloc("xT", XT_OFF, [128, 8, S], F32, nres=32)
        XR = [[XR[c * 4 + t] for t in range(4)] for c in range(8)]
        HT, HR = A.alloc("hT", HT_OFF, [128, 8, S + 2], BF16, nres=4)
        YG, YR = A.alloc("yg", YG_OFF, [128, 8, S], BF16, nres=32)
        YR = [[YR[c * 4 + t] for t in range(4)] for c in range(8)]
        CF, CFR = A.alloc("cF", CF_OFF, [128, NCF], F32)
        CB, CBR = A.alloc("cB", CB_OFF, [128, NCB], BF16)
        PR, PRR = A.alloc("par", PR_OFF, [128, L, NPAR], F32)
        GW, GWR = A.alloc("gw", GW_OFF, [17, L, 256], F32)

        identF = CF[:, 0:128]
        onesF = CF[:, 128:256]
        TriM = CF[:, 256:384]
        UTm = CF[:, 384:512]
        mask01 = CF[:, 512:640]
        bias_init = CF[:, 640:1152]
        identB = CB[:, 0:128]
        onesB = CB[:, 128:256]
        Wv = CB[:, 256:512].rearrange("p (a b) -> p a b", a=2)
        Cmask = CB[:, 512:1024].rearrange("p (a b) -> p a b", a=2)

        DMA("sp", CF, d_cf, writes=[CFR])
        DMA("sp", PR, d_par, writes=[PRR])
        DMA("sp", GW, d_gw, writes=[GWR])
        for tt in range(4):
            for c in range(8):
                DMA("sp", XT[:, c, tt * 512: tt * 512 + 512], d_x[c][:, tt * 512: tt * 512 + 512], writes=[XR[c][tt]])
        DMA("pool", CB, d_cb, writes=[CBR])
        MSET("dve", HT[:, :, 0:2], 0.0, [HR[0]])

        def rmsnorm(gcol, bank0, final=False):
            TZ = SCR + 50 * 1024
            sq, sqR = A.alloc("n_sq", TZ, [128, 2, 512], BF16, nres=2)
            ln, lnR = A.alloc("n_ln", TZ + 2048, [128, 512], F32)
            k = 0
            for tt in range(4):
                tok = slice(tt * 512, tt * 512 + 512)
                b = bank0 + (tt % 2)
                for c in range(8):
                    ACT(sq[:, k % 2, :], XT[:, c, tok], AF.Square, [XR[c][tt]], [sqR[k % 2]])
                    MM(PS[b][:, :], onesB, sq[:, k % 2, :], c == 0, c == 7, [sqR[k % 2], CBR], [PSR[b]])
                    k += 1
                ACT(ln, PS[b][:, :], AF.Ln, [PSR[b]], [lnR], bias=1e-6, scale=1.0 / D)
                ACT(ln, ln, AF.Exp, [lnR], [lnR], scale=-0.5)
                for c in range(8):
                    if not final:
                        STT("dve", HT[:, c, 2 + tt * 512: 2 + tt * 512 + 512], XT[:, c, tok], PR[:, 0, gcol + c: gcol + c + 1],
                            ln, ALU.mult, ALU.mult, [XR[c][tt], lnR, PRR], [HR[tt]])
                    else:
                        STT("dve", XT[:, c, tok], XT[:, c, tok], PR[:, 0, gcol + c: gcol + c + 1],
                            ln, ALU.mult, ALU.mult, [lnR, PRR], [XR[c][tt]])
                        DMA("sp", d_out[c][:, tok], XT[:, c, tok], reads=[XR[c][tt]])

        def rmsnorm_l(l, which, bank0):
            base = 0 if which == "mix" else 8
            TZ = SCR + 50 * 1024
            sq, sqR = A.alloc("n_sq", TZ, [128, 2, 512], BF16, nres=2)
            ln, lnR = A.alloc("n_ln", TZ + 2048, [128, 512], F32)
            k = 0
            for tt in range(4):
                tok = slice(tt * 512, tt * 512 + 512)
                b = bank0 + (tt % 2)
                for c in range(8):
                    ACT(sq[:, k % 2, :], XT[:, c, tok], AF.Square, [XR[c][tt]], [sqR[k % 2]])
                    MM(PS[b][:, :], onesB, sq[:, k % 2, :], c == 0, c == 7, [sqR[k % 2], CBR], [PSR[b]])
                    k += 1
                ACT(ln, PS[b][:, :], AF.Ln, [PSR[b]], [lnR], bias=1e-6, scale=1.0 / D)
                ACT(ln, ln, AF.Exp, [lnR], [lnR], scale=-0.5)
                for c in range(8):
                    STT("dve", HT[:, c, 2 + tt * 512: 2 + tt * 512 + 512], XT[:, c, tok],
                        PR[:, l, base + c: base + c + 1], ln, ALU.mult, ALU.mult,
                        [XR[c][tt], lnR, PRR], [HR[tt]])

        for l in range(L):
          try:
            par = lambda col, n=1: PR[:, l, col: col + n]
            P.tag = 'norm'
            rmsnorm_l(l, "mix", 6)
            P.tag = 'A'

            if stage == -1:
                for c in range(8):
                    for tt in range(4):
                        CP("dve", XT[:, c, tt * 512: tt * 512 + 512], HT[:, c, 2 + tt * 512: 2 + tt * 512 + 512], [HR[tt]], [XR[c][tt]])
                break
            WA, WAR = A.alloc("wA", SCR, [128, 8, 512], BF16)
            DMA("pool", WA, d_win[l][:, :, 0:512], writes=[WAR])
            hA, hAR = A.alloc("hA", SCR + 8192, [128, 2, 30 + S], BF16, nres=2)
            Dg, DgR = A.alloc("Dg", SCR + 16512, [128, 2, 31, 128], BF16)
            sig, sigR = A.alloc("sig", SCR + 32384, [128, 2, 512], F32, nres=2)
            cv, cvR = A.alloc("cv", SCR + 36480, [128, 2, 512], F32)
            csq, csqR = A.alloc("csq", SCR + 40576, [128, 2, 512], BF16)
            mean, meanR = A.alloc("mean", SCR + 44672, [128, 512], F32)
            b1, b1R = A.alloc("b1", SCR + 46720, [128, 512], F32)
            b2, b2R = A.alloc("b2", SCR + 48768, [128, 512], F32)
            for c in range(2):
                MSET("pool", hA[:, c, 0:30], 0.0, [hAR[c]])
                cw = PR[:, l, 16 + c * 31: 16 + c * 31 + 31]
                TT("dve", Dg[:, c, :, :], identB[:, None, :].broadcast_to([128, 31, 128]),
                   cw[:, :, None].broadcast_to([128, 31, 128]), ALU.mult, [CBR, PRR], [DgR])
            for tt in range(4):
                tok = slice(tt * 512, tt * 512 + 512)
                hsl = slice(2 + tt * 512, 2 + tt * 512 + 512)
                for c in range(2):
                    for kc in range(8):
                        MM(PS[c][:, :], WA[:, kc, c * 128: c * 128 + 128], HT[:, kc, hsl], kc == 0, kc == 7,
                           [WAR, HR[tt]], [PSR[c]])
                    for kc in range(8):
                        MM(PS[2 + c][:, :], WA[:, kc, 256 + c * 128: 256 + c * 128 + 128], HT[:, kc, hsl], kc == 0, kc == 7,
                           [WAR, HR[tt]], [PSR[2 + c]])
                    ACT(sig[:, c, :], PS[2 + c][:, :], AF.Sigmoid, [PSR[2 + c]], [sigR[c]])
                    TT("dve", hA[:, c, 30 + tt * 512: 30 + tt * 512 + 512], PS[c][:, :], sig[:, c, :], ALU.mult,
                       [PSR[c], sigR[c]], [hAR[c]])
                for c in range(2):
                    for i in range(31):
                        MM(PS[4 + c][:, :], Dg[:, c, i, :], hA[:, c, tt * 512 + i: tt * 512 + i + 512], i == 0, i == 30,
                           [DgR, hAR[c]], [PSR[4 + c]])
                    ACT(cv[:, c, :], PS[4 + c][:, :], AF.Identity, [PSR[4 + c], PRR], [cvR], bias=par(78 + c))
                ACT(csq, cv, AF.Square, [cvR], [csqR])
                for c in range(2):
                    MM(PS[6][:, :], onesF, cv[:, c, :], c == 0, c == 1, [cvR, CFR], [PSR[6]])
                for c in range(2):
                    MM(PS[7][:, :], onesB, csq[:, c, :], c == 0, c == 1, [csqR, CBR], [PSR[7]])
                TS("dve", mean, PS[6][:, :], 1.0 / 256, ALU.mult, [PSR[6]], [meanR])
                TT("dve", b1, mean, mean, ALU.mult, [meanR], [b1R])
                STT("dve", b2, PS[7][:, :], 1.0 / 256, b1, ALU.mult, ALU.subtract, [PSR[7], b1R], [b2R])
                ACT(b1, b2, AF.Ln, [b2R], [b1R], bias=1e-5)
                ACT(b2, b1, AF.Exp, [b1R], [b2R], scale=-0.5)
                for c in range(2):
                    TT("dve", cv[:, c, :], cv[:, c, :], mean, ALU.subtract, [cvR, meanR], [cvR])
                    TT("dve", cv[:, c, :], cv[:, c, :], b2, ALU.mult, [cvR, b2R], [cvR])
                    ACT(YG[:, c, tok], cv[:, c, :], AF.Silu, [cvR, PRR], [YR[c][tt]], bias=par(82 + c), scale=par(80 + c))

            if stage == 11:
                for c in range(8):
                    for tt in range(4):
                        CP("dve", XT[:, c, tt * 512: tt * 512 + 512], YG[:, c, tt * 512: tt * 512 + 512], [YR[c][tt]], [XR[c][tt]])
                break
            P.tag = 'C'
            WC, WCR = A.alloc("wC", SCR, [128, 8, 1552], BF16, nres=3)
            for pi, (c0, c1) in enumerate(((0, 512), (512, 1024), (1024, 1552))):
                DMA("pool", WC[:, :, c0:c1], d_win[l][:, :, 1280 + c0: 1280 + c1], writes=[WCR[pi]])
            o = SCR + 24832
            qT2, qTR2 = A.alloc("qT", o, [128, 2, 2, 512], BF16, nres=2); o += 4096
            kT2, kTR2 = A.alloc("kT", o, [128, 2, 2, 512], BF16, nres=2); o += 4096
            rs, rsR = A.alloc("rs", o, [128, 4, 512], BF16); o += 4096
            vtok2, vtokR2 = A.alloc("vtok", o, [128, 2, 4, 512], BF16, nres=8); o += 8192
            g162, g16R2 = A.alloc("g16", o, [17, 2, 512], F32, nres=2); o += 4096
            la2, laR2 = A.alloc("la", o, [128, 2, 256], F32, nres=2); o += 2048
            eGn2, eGnR2 = A.alloc("eGn", o, [128, 2, 256], F32, nres=2); o += 2048
            kd2, kdR2 = A.alloc("kd", o, [128, 2, 256], BF16, nres=2); o += 1024
            eG2, eGR2 = A.alloc("eG", o, [128, 2, 2, 128], F32, nres=2); o += 2048
            eGi2, eGiR2 = A.alloc("eGi", o, [128, 2, 2, 128], F32, nres=2); o += 2048
            qg2, qgR2 = A.alloc("qg", o, [128, 2, 2, 128], BF16, nres=2); o += 1024
            kg2, kgR2 = A.alloc("kg", o, [128, 2, 2, 128], BF16, nres=2); o += 1024
            Am, AmR = A.alloc("Am", o, [128, 4, 128], BF16); o += 1024
            Sf, SfR = A.alloc("Sf", o, [128, 2, 128], F32); o += 1024
            Sb, SbR = A.alloc("Sb", o, [128, 2, 2, 128], BF16, nres=2); o += 1024
            osq, osqR = A.alloc("osq", o, [128, 512], BF16); o += 1024
            orr, orrR = A.alloc("orr", o, [128, 512], F32); o += 2048
            assert o <= SCR + SCR_SZ, o - SCR
            MSET("dve", g162, 1.0, g16R2)
            MSET("dve", Sf, 0.0, [SfR])
            pb = 0
            for tc in range(4):
                tok = slice(tc * 512, tc * 512 + 512)
                hsl = slice(2 + tc * 512, 2 + tc * 512 + 512)
                s_ = tc % 2
                qT, qTR = qT2[:, s_, :, :], qTR2[s_]
                kT, kTR = kT2[:, s_, :, :], kTR2[s_]
                vtok, vtokR = vtok2[:, s_, :, :], vtokR2[s_ * 4: s_ * 4 + 4]
                g16, g16R = g162[:, s_, :], g16R2[s_]
                for (dst, dstR, col0, nch) in ((qT, qTR, 0, 2), (kT, kTR, 256, 2)):
                    for c in range(nch):
                        b = pb % 2; pb += 1
                        for kc in range(8):
                            MM(PS[b][:, :], WC[:, kc, col0 + c * 128: col0 + c * 128 + 128], HT[:, kc, hsl], kc == 0, kc == 7,
                               [WCR[0], HR[tc]], [PSR[b]])
                        ACT(dst[:, c, :], PS[b][:, :], AF.Copy, [PSR[b]], [dstR])
                for c in range(4):
                    b = pb % 2; pb += 1
                    for kc in range(8):
                        MM(PS[b][:, :], WC[:, kc, 1040 + c * 128: 1040 + c * 128 + 128], HT[:, kc, hsl], kc == 0, kc == 7,
                           [WCR[2], HR[tc]], [PSR[b]])
                    ACT(rs[:, c, :], PS[b][:, :], AF.Silu, [PSR[b]], [rsR])
                b = pb % 2; pb += 1
                for kc in range(8):
                    MM(PS[b][0:16, :], WC[:, kc, 1024:1040], HT[:, kc, hsl], kc == 0, kc == 7, [WCR[2], HR[tc]], [PSR[b]])
                ACT(g16[0:16, :], PS[b][0:16, :], AF.Copy, [PSR[b]], [g16R])
                for t in range(4):
                    b = pb % 2; pb += 1
                    h128 = slice(2 + tc * 512 + t * 128, 2 + tc * 512 + t * 128 + 128)
                    for kc in range(8):
                        MM(PS[b][:, :], HT[:, kc, h128], WC[:, kc, 512:1024], kc == 0, kc == 7, [WCR[1], HR[tc]], [PSR[b]])
                    ACT(vtok[:, t, :], PS[b][:, :], AF.Copy, [PSR[b]], [vtokR[t]])
                if cstop == 1:
                    raise StopBuild()
                for t in range(4):
                    gt = tc * 4 + t
                    g_ = gt % 2
                    la, laR = la2[:, g_, :], laR2[g_]
                    eGn, eGnR = eGn2[:, g_, :], eGnR2[g_]
                    kd, kdR = kd2[:, g_, :], kdR2[g_]
                    eG, eGR = eG2[:, g_, :, :], eGR2[g_]
                    eGi, eGiR = eGi2[:, g_, :, :], eGiR2[g_]
                    qg, qgR = qg2[:, g_, :, :], qgR2[g_]
                    kg, kgR = kg2[:, g_, :, :], kgR2[g_]
                    t128 = slice(t * 128, t * 128 + 128)
                    h128 = slice(2 + tc * 512 + t * 128, 2 + tc * 512 + t * 128 + 128)
                    MM(PS[2][:, 0:256], g16[0:17, t128], GW[0:17, l, :], True, True, [g16R, GWR], [PSR[2]])
                    ACT(la, PS[2][:, 0:256], AF.Exp, [PSR[2]], [laR], scale=-1.0)
                    ACT(la, la, AF.Ln, [laR], [laR], bias=1.0)
                    MM(PS[2][:, 256:512], UTm, la, True, True, [laR, CFR], [PSR[2]])
                    ACT(eGn, PS[2][:, 256:512], AF.Exp, [PSR[2]], [eGnR])
                    if cstop == 2:
                        raise StopBuild()
                    for kc in range(8):
                        MM(PS[3][:, 0:256], HT[:, kc, h128], WC[:, kc, 256:512], kc == 0, kc == 7, [WCR[0], HR[tc]], [PSR[3]])
                    TT("dve", kd, PS[3][:, 0:256], eGn, ALU.mult, [PSR[3], eGnR], [kdR])
                    if cstop == 3:
                        raise StopBuild()
                    for c in range(2):
                        MM(PS[3][:, 256 + c * 128: 256 + c * 128 + 128], la[:, c * 128: c * 128 + 128], TriM, True, True,
                           [laR, CFR], [PSR[3]])
                    gT = PS[3][:, 256:512].rearrange("p (a b) -> p a b", a=2)
                    ACT(eG, gT, AF.Exp, [PSR[3]], [eGR])
                    ACT(eGi, gT, AF.Exp, [PSR[3]], [eGiR], scale=-1.0)
                    STT("dve", qg, qT[:, :, t128], 0.125, eG, ALU.mult, ALU.mult, [qTR, eGR], [qgR])
                    TT("dve", kg, kT[:, :, t128], eGi, ALU.mult, [kTR, eGiR], [kgR])
                    if cstop == 4:
                        raise StopBuild()
                    for h in range(4):
                        hp = slice((h % 2) * 64, (h % 2) * 64 + 64)
                        ab = 4 if h % 2 == 0 else 7
                        MM(PS[ab][:, (h // 2) * 128: (h // 2) * 128 + 128], kg[hp, h // 2, :], qg[hp, h // 2, :], True, True,
                           [kgR, qgR], [PSR[ab]])
                    for par_ in range(2):
                        ab = 4 if par_ == 0 else 7
                        TT("dve", Am[:, par_:4:2, :], PS[ab][:, 0:256].rearrange("p (a b) -> p a b", a=2),
                           mask01[:, None, :].broadcast_to([128, 2, 128]), ALU.mult, [PSR[ab], CFR], [AmR])
                    if cstop == 5:
                        raise StopBuild()
                    for h in range(4):
                        hp = slice((h % 2) * 64, (h % 2) * 64 + 64)
                        MM(PS[5][hp, (h // 2) * 128: (h // 2) * 128 + 128], kd[:, h * 64: h * 64 + 64],
                           vtok[:, t, h * 128: h * 128 + 128], True, True, [kdR, vtokR[t]], [PSR[5]])
                    if cstop == 6:
                        raise StopBuild()
                    sb_cur = gt % 2
                    for h in range(4):
                        hp = slice((h % 2) * 64, (h % 2) * 64 + 64)
                        MM(PS[6][:, h * 128: h * 128 + 128], vtok[:, t, h * 128: h * 128 + 128], Am[:, h, :], True, gt == 0,
                           [vtokR[t], AmR], [PSR[6]])
                        if gt > 0:
                            MM(PS[6][:, h * 128: h * 128 + 128], Sb[hp, sb_cur, h // 2, :], qg[hp, h // 2, :], False, True,
                               [SbR[sb_cur], qgR], [PSR[6]])
                    if cstop == 7:
                        raise StopBuild()
                    for c in range(2):
                        STT("dve", Sf[:, c, :], Sf[:, c, :], eG[:, c, 127:128], PS[5][:, c * 128: c * 128 + 128],
                            ALU.mult, ALU.add, [SfR, eGR, PSR[5]], [SfR])
                    CP("dve", Sb[:, 1 - sb_cur, :, :], Sf, [SfR], [SbR[1 - sb_cur]])
                    if cstop == 8:
                        raise StopBuild()
                    ACT(osq, PS[6][:, :], AF.Square, [PSR[6]], [osqR])
                    MM(PS[7][:, :], onesB, osq, True, True, [osqR, CBR], [PSR[7]])
                    ACT(orr, PS[7][:, :], AF.Ln, [PSR[7]], [orrR], bias=1e-6, scale=1.0 / 128)
                    ACT(orr, orr, AF.Exp, [orrR], [orrR], scale=-0.5)
                    TT("dve", orr, PS[6][:, :], orr, ALU.mult, [PSR[6], orrR], [orrR])
                    for h in range(4):
                        STT("dve", YG[:, 4 + h, gt * 128: gt * 128 + 128], orr[:, h * 128: h * 128 + 128], par(86 + h),
                            rs[:, h, t128], ALU.mult, ALU.mult, [orrR, rsR, PRR], [YR[4 + h][tc]])

            if stage == 12:
                for c in range(8):
                    for tt in range(4):
                        CP("dve", XT[:, c, tt * 512: tt * 512 + 512], YG[:, c, tt * 512: tt * 512 + 512], [YR[c][tt]], [XR[c][tt]])
                break
            P.tag = 'B'
            WB, WBR = A.alloc("wB", SCR, [128, 8, 768], BF16, nres=3)
            for pi in range(3):
                DMA("pool", WB[:, :, pi * 256: pi * 256 + 256], d_win[l][:, :, 512 + pi * 256: 768 + pi * 256], writes=[WBR[pi]])
            o = SCR + 12288
            QZ, QZR = A.alloc("QZ", o, [128, 4, S], BF16, nres=16); o += 16384
            QZR = [[QZR[h * 4 + t] for t in range(4)] for h in range(4)]
            KZ, KZR = A.alloc("KZ", o, [128, 4, S], BF16, nres=16); o += 16384
            KZR = [[KZR[h * 4 + t] for t in range(4)] for h in range(4)]
            VA, VAR = A.alloc("VA", o, [128, 16, 2, 192], BF16, nres=16); o += 12288
            ball, ballR = A.alloc("ball", o, [128, 16, 2, 72], BF16, nres=16); o += 4608
            km, kmR = A.alloc("km", o, [128, 2, 8], F32, nres=4); o += 64
            kmb, kmbR = A.alloc("kmb", o, [128, 4, 8], BF16, nres=4); o += 64
            gs, gsR = A.alloc("gs", o, [128, 4, 8], F32); o += 128
            cmpb, cmpR = A.alloc("cmp", o, [128, 4, 8, 8], F32); o += 1024
            rank, rankR = A.alloc("rank", o, [128, 4, 8], F32); o += 128
            pT, pTR = A.alloc("pT", o, [128, 3, 512], BF16, nres=3); o += 3072
            msq, msqR = A.alloc("msq", o, [128, 2, 512], BF16, nres=2); o += 2048
            mrs, mrsR = A.alloc("mrs", o, [128, 512], F32); o += 2048
            assert o <= SCR + SCR_SZ, o - SCR
            MSET("dve", VA[:, :, :, 64:128], 1.0, VAR)
            MSET("dve", kmb, 0.0, kmbR)
            MSET("dve", ball, -BIG, ballR)
            for gt in range(16):
                own = gt // 2
                MSET("pool", ball[:, gt, :, own: 72: 64], 0.0, [ballR[gt]])
            for h in range(4):
                oh = slice((1 - h % 2) * 64, (1 - h % 2) * 64 + 64)
                MSET("dve", QZ[oh, h, :], 0.0, QZR[h])
                MSET("dve", KZ[oh, h, :], 0.0, KZR[h])
                r0 = (1 - h % 2) * 64
                DMA("pool", KZ[r0: r0 + 8, h, :], d_e8, writes=KZR[h])
            pb = 0
            for tt in range(4):
                hsl = slice(2 + tt * 512, 2 + tt * 512 + 512)
                tok = slice(tt * 512, tt * 512 + 512)
                for c in range(2):
                    b = pb % 2; pb += 1
                    for kc in range(8):
                        MM(PS[b][:, :], WB[:, kc, c * 128: c * 128 + 128], HT[:, kc, hsl], kc == 0, kc == 7, [WBR[0], HR[tt]], [PSR[b]])
                    CP("dve", QZ[0:64, 2 * c, tok], PS[b][0:64, :], [PSR[b]], [QZR[2 * c][tt]])
                    CP("dve", QZ[64:128, 2 * c + 1, tok], PS[b][64:128, :], [PSR[b]], [QZR[2 * c + 1][tt]])
                for c in range(2):
                    b = pb % 2; pb += 1
                    for kc in range(8):
                        MM(PS[b][:, :], WB[:, kc, 256 + c * 128: 256 + c * 128 + 128], HT[:, kc, hsl], kc == 0, kc == 7,
                           [WBR[1], HR[tt]], [PSR[b]])
                    CP("dve", KZ[0:64, 2 * c, tok], PS[b][0:64, :], [PSR[b]], [KZR[2 * c][tt]])
                    CP("dve", KZ[64:128, 2 * c + 1, tok], PS[b][64:128, :], [PSR[b]], [KZR[2 * c + 1][tt]])
                    RSUM("dve", km[:, c, 2 * tt: 2 * tt + 2], PS[b][:, :].rearrange("p (a b) -> p a b", a=2), [PSR[b]], [kmR[tt]])
                for par_ in range(2):
                    hp_ = slice(par_ * 64, par_ * 64 + 64)
                    CP("dve", kmb[hp_, par_:4:2, 2 * tt: 2 * tt + 2], km[hp_, :, 2 * tt: 2 * tt + 2], [kmR[tt]], [kmbR[tt]])
                for t in range(4):
                    gt = tt * 4 + t
                    b = pb % 2; pb += 1
                    h128 = slice(2 + gt * 128, 2 + gt * 128 + 128)
                    for kc in range(8):
                        MM(PS[b][:, 0:256], HT[:, kc, h128], WB[:, kc, 512:768], kc == 0, kc == 7, [WBR[2], HR[tt]], [PSR[b]])
                    src = PS[b][:, 0:256].rearrange("p (a w c) -> p a w c", a=2, w=2)
                    dstv = VA[:, gt, :, :].rearrange("p a (w c) -> p a w c", w=3)[:, :, 0:3:2, :]
                    CP("dve", dstv, src, [PSR[b]], [VAR[gt]])
            PSB3 = PS[7][:, :].bitcast(BF16)
            for gt in range(16):
                own = gt // 2
                tt = gt // 4
                t128 = slice(gt * 128, gt * 128 + 128)
                if own > 0:
                    for h in range(4):
                        MM(PS[4][:, h * 8: h * 8 + 8], QZ[:, h, t128], kmb[:, h, :], True, True,
                           [QZR[h][tt]] + [kmbR[i] for i in range((own - 1) // 2 + 1)], [PSR[4]])
                    g3 = PS[4][:, 0:32].rearrange("p (a b) -> p a b", a=4)
                    ACT(gs[:, :, 0:own], g3[:, :, 0:own], AF.Copy, [PSR[4]], [gsR])
                    gv = gs[:, :, 0:own]
                    TT("dve", cmpb[:, :, 0:own, 0:own], gv[:, :, None, :].broadcast_to([128, 4, own, own]),
                       gv[:, :, :, None].broadcast_to([128, 4, own, own]), ALU.is_gt, [gsR], [cmpR])
                    RSUM("dve", rank[:, :, 0:own], cmpb[:, :, 0:own, 0:own], [cmpR], [rankR])
                    for par_ in range(2):
                        c0 = 64 if par_ == 0 else 0
                        TS("dve", ball[:, gt, :, c0: c0 + own], rank[:, par_:4:2, 0:own], 2.5, ALU.is_ge,
                           [rankR], [ballR[gt]], s2=-BIG, op1=ALU.mult)
                for hc in range(2):
                    col = (hc * 4 + gt % 4) * 128
                    TR(PSB3[0:72, col: col + 128], ball[:, gt, hc, :], identB, [ballR[gt], CBR], [PSR[7]])
                if gt % 4 == 3:
                    tok = slice(tt * 512, tt * 512 + 512)
                    for hc in range(2):
                        CP("dve", QZ[0:8, 2 * hc + 1, tok], PSB3[0:8, hc * 512: hc * 512 + 512], [PSR[7]], [QZR[2 * hc + 1][tt]])
                        CP("dve", QZ[64:72, 2 * hc, tok], PSB3[64:72, hc * 512: hc * 512 + 512], [PSR[7]], [QZR[2 * hc][tt]])
            sb_i = 0
            g_i = 0
            for hc in range(2):
                for qc in range(4):
                    qtok = slice(qc * 512, qc * 512 + 512)
                    obs = (0, 1) if g_i % 2 == 0 else (5, 6)
                    g_i += 1
                    nk = 4 * qc + 4
                    for par_ in range(2):
                        h = 2 * hc + par_
                        ob = obs[par_]
                        for kt in range(nk):
                            sbk = 2 + (sb_i % 3)
                            pslot = sb_i % 3
                            sb_i += 1
                            kb = kt // 2
                            diag = kb in (2 * qc, 2 * qc + 1)
                            c0 = 256 if kb == 2 * qc + 1 else 0
                            cs = slice(c0, 512)
                            MM(PS[sbk][:, cs], KZ[:, h, kt * 128: kt * 128 + 128], QZ[:, h, qc * 512 + c0: qc * 512 + 512], True, not diag,
                               [KZR[h][kt // 4], QZR[h][qc]], [PSR[sbk]])
                            if diag:
                                qb = kb - 2 * qc
                                MM(PS[sbk][:, qb * 256: qb * 256 + 256], identB, Cmask[:, kt % 2, :], False, True,
                                   [CBR], [PSR[sbk]])
                            ACT(pT[:, pslot, cs], PS[sbk][:, cs], AF.Exp, [PSR[sbk]], [pTR[pslot]], scale=0.125)
                            MM(PS[ob][:, cs], VA[:, kt, hc, par_ * 64: par_ * 64 + 128], pT[:, pslot, cs], kt == 0, kt == nk - 1,
                               [VAR[kt], pTR[pslot]], [PSR[ob]])
                        ACT(msq[:, par_, :], PS[ob][:, :], AF.Square, [PSR[ob]], [msqR[par_]])
                    MM(PS[7][:, :], Wv[:, 0, :], msq[:, 0, :], True, False, [msqR[0], CBR], [PSR[7]])
                    MM(PS[7][:, :], Wv[:, 1, :], msq[:, 1, :], False, True, [msqR[1], CBR], [PSR[7]])
                    ACT(mrs, PS[7][:, :], AF.Ln, [PSR[7]], [mrsR])
                    ACT(mrs, mrs, AF.Exp, [mrsR], [mrsR], scale=-0.5)
                    for par_ in range(2):
                        hp = slice(par_ * 64, par_ * 64 + 64)
                        STT("dve", YG[hp, 2 + hc, qtok], PS[obs[par_]][hp, :], PR[hp, l, 84 + hc: 85 + hc], mrs[hp, :], ALU.mult, ALU.mult,
                            [PSR[obs[par_]], mrsR, PRR], [YR[2 + hc][qc]])

            if stage == 1 and l == 0:
                for c in range(8):
                    for tt in range(4):
                        CP("dve", XT[:, c, tt * 512: tt * 512 + 512], YG[:, c, tt * 512: tt * 512 + 512], [YR[c][tt]], [XR[c][tt]])
                break

            P.tag = 'O'
            WO, WOR = A.alloc("wO", SCR, [128, 8, 1024], BF16, nres=4)
            for pi in range(4):
                DMA("pool", WO[:, :, pi * 256: pi * 256 + 256], d_wout[l][:, :, pi * 256: pi * 256 + 256], writes=[WOR[pi]])
            pb = 0
            for dc in range(8):
                for tt in range(4):
                    tok = slice(tt * 512, tt * 512 + 512)
                    b = pb % 4; pb += 1
                    for kc in range(8):
                        MM(PS[b][:, :], WO[:, kc, dc * 128: dc * 128 + 128], YG[:, kc, tok], kc == 0, kc == 7,
                           [WOR[dc // 2], YR[kc][tt]], [PSR[b]])
                    TT("dve", XT[:, dc, tok], XT[:, dc, tok], PS[b][:, :], ALU.add, [PSR[b], XR[dc][tt]], [XR[dc][tt]])
            if stage == 2 and l == 0:
                break

            P.tag = 'norm'
            rmsnorm_l(l, "ffn", 6)
            P.tag = 'F'
            WU, WUR = A.alloc("wU", SCR, [128, 3, 8, 256], BF16, nres=3)
            WD, WDR = A.alloc("wD", SCR + 12288, [128, 8, 1024], BF16, nres=8)
            TM, TMR = A.alloc("ftmp", SCR + 28672, [128, 3, 5, 416], F32, nres=15)
            TMR = [[TMR[a * 5 + k] for k in range(5)] for a in range(3)]
            it = 0
            dpb = 0
            for rnd in ROUNDS:
                for si, j in enumerate(rnd):
                    us = j % 3
                    DMA("pool", WU[:, us, :, :], d_wup[l][j], writes=[WUR[us]])
                    DMA("pool", WD[:, si, :], d_wdn[l][j], writes=[WDR[si]])
                    for (t0, t1) in FFN_TILES:
                        w = t1 - t0
                        a = it % 3
                        bg = (it % 3) * 2
                        bv = bg + 1
                        it += 1
                        hres = [HR[i] for i in tiles_of(max(t0 - 2, 0), t1)]
                        for kc in range(8):
                            MM(PS[bg][:, 0:w + 2], WU[:, us, kc, 0:128], HT[:, kc, t0: t0 + w + 2], kc == 0, kc == 7,
                               [WUR[us]] + hres, [PSR[bg]])
                        for kc in range(8):
                            MM(PS[bv][:, 0:w + 2], WU[:, us, kc, 128:256], HT[:, kc, t0: t0 + w + 2], kc == 0, kc == 7,
                               [WUR[us]] + hres, [PSR[bv]])
                        fw = lambda ch, i: PR[:, l, 90 + ch * 3 + i: 90 + ch * 3 + i + 1]
                        fb = lambda ch: PR[:, l, 222 + ch: 223 + ch]
                        T = [TM[:, a, k, 0:w] for k in range(5)]
                        TRs = TMR[a]
                        ACT(T[0], PS[bg][:, 2: 2 + w], AF.Identity, [PSR[bg], PRR], [TRs[0]], bias=fb(j), scale=fw(j, 2))
                        STT("dve", T[1], PS[bg][:, 1: 1 + w], fw(j, 1), T[0], ALU.mult, ALU.add, [PSR[bg], TRs[0], PRR], [TRs[1]])
                        STT("dve", T[0], PS[bg][:, 0: w], fw(j, 0), T[1], ALU.mult, ALU.add, [PSR[bg], TRs[1], PRR], [TRs[0]])
                        ACT(T[1], T[0], AF.Silu, [TRs[0]], [TRs[1]])
                        ACT(T[2], PS[bv][:, 2: 2 + w], AF.Identity, [PSR[bv], PRR], [TRs[2]], bias=fb(22 + j), scale=fw(22 + j, 2))
                        STT("dve", T[3], PS[bv][:, 1: 1 + w], fw(22 + j, 1), T[2], ALU.mult, ALU.add,
                            [PSR[bv], TRs[2], PRR], [TRs[3]])
                        STT("dve", T[2], PS[bv][:, 0: w], fw(22 + j, 0), T[3], ALU.mult, ALU.add,
                            [PSR[bv], TRs[3], PRR], [TRs[2]])
                        TT("pool", YG[:, si, t0:t1], T[1], T[2], ALU.mult, [TRs[1], TRs[2]],
                           [YR[si][i] for i in tiles_of(t0, t1)])
                for tt in range(4):
                    tok = slice(tt * 512, tt * 512 + 512)
                    for dc in range(8):
                        b = 6 + (dpb % 2); dpb += 1
                        for si in range(len(rnd)):
                            MM(PS[b][:, :], WD[:, si, dc * 128: dc * 128 + 128], YG[:, si, tok], si == 0, si == len(rnd) - 1,
                               [WDR[si], YR[si][tt]], [PSR[b]])
                        TT("dve", XT[:, dc, tok], XT[:, dc, tok], PS[b][:, :], ALU.add, [PSR[b], XR[dc][tt]], [XR[dc][tt]])
            if stage == 3 and l == 0:
                break
          except StopBuild:
            break

        if stage == 0:
            rmsnorm(266, 6, final=True)
        else:
            for c in range(8):
                DMA("sp", d_out[c], XT[:, c, :], reads=XR[c])
        P.finish("sp")
        P.emit()
        global LAST_PROG
        LAST_PROG = P
    return nc


def _consts():
    cf = np.zeros((128, NCF), np.float32)
    j = np.arange(128)[:, None]
    i = np.arange(128)[None, :]
    cf[:, 0:128] = np.eye(128)
    cf[:, 128:256] = 1.0
    cf[:, 256:384] = np.where(j <= i, -1.0 / 16, 0.0)
    cf[:, 384:512] = np.where(j > i, -1.0 / 16, 0.0)
    cf[:, 512:640] = np.where(j <= i, 1.0, 0.0)
    bi = np.full((16, 4, 8), -BIG, np.float32)
    for gt in range(16):
        bi[gt, :, gt // 2] = 0.0
    cf[:, 640:1152] = bi.reshape(1, 512)
    cb = np.zeros((128, NCB), np.float32)
    cb[:, 0:128] = np.eye(128)
    cb[:, 128:256] = 1.0
    wv = np.zeros((128, 2, 128), np.float32)
    wv[0:64, 0, 0:64] = 1.0 / 64
    wv[64, 0, 0:64] = 1e-6
    wv[64:128, 1, 64:128] = 1.0 / 64
    wv[0, 1, 64:128] = 1e-6
    cb[:, 256:512] = wv.reshape(128, 256)
    cm = np.zeros((128, 2, 256), np.float32)
    k = np.arange(128)[:, None]
    q = np.arange(256)[None, :]
    for par in range(2):
        cm[:, par, :] = np.where(par * 128 + k > q, -BIG, 0.0)
    cb[:, 512:1024] = cm.reshape(128, 512)
    e8 = np.zeros((8, S), np.float32)
    for n in range(8):
        e8[n, n * 256:(n + 1) * 256] = 1.0
    return cf, cb, e8


def _prep_shared(inp):
    f = lambda a: np.ascontiguousarray(a, dtype=np.float32)
    w_in = f(inp["w_in"].reshape(L, 8, 128, 2832).transpose(0, 2, 1, 3))
    w_out = f(inp["w_out"].reshape(L, 8, 128, 1024).transpose(0, 2, 1, 3))
    wu = inp["ffn_w_up"].reshape(L, 8, 128, 2, 22, 128)
    w_up = f(wu.transpose(0, 4, 2, 1, 3, 5).reshape(L, 22, 128, 8, 256))
    w_dn = f(inp["ffn_w_down"].reshape(L, 22, 128, 1024))
    par = np.zeros((128, L, NPAR), np.float32)
    for l in range(L):
        par[:, l, 0:8] = inp["norm_mix_g"][l].reshape(8, 128).T
        par[:, l, 8:16] = inp["norm_ffn_g"][l].reshape(8, 128).T
        par[:, l, 16:78] = inp["conv_w"][l].T.reshape(2, 128, 31).transpose(1, 0, 2).reshape(128, 62)
        par[:, l, 78:80] = inp["conv_b"][l].reshape(2, 128).T
        par[:, l, 80:82] = inp["conv_ln_g"][l].reshape(2, 128).T
        par[:, l, 82:84] = inp["conv_ln_b"][l].reshape(2, 128).T
        par[:, l, 84:86] = inp["moba_out_g"][l].reshape(2, 128).T
        par[:, l, 86:90] = inp["gla_out_g"][l].reshape(4, 128).T
        par[:, l, 90:222] = inp["ffn_conv_w"][l].T.reshape(44, 128, 3).transpose(1, 0, 2).reshape(128, 132)
        par[:, l, 222:266] = inp["ffn_conv_b"][l].reshape(44, 128).T
        par[:, l, 266:274] = inp["final_g"].reshape(8, 128).T
    gw = np.zeros((17, L, 256), np.float32)
    for l in range(L):
        gw[0:16, l] = inp["gla_gate_w"][l]
        gw[16, l] = inp["gla_gate_b"][l]
    cf, cb, ce = _consts()
    return {"w_in": w_in, "w_out": w_out, "w_up": w_up, "w_dn": w_dn, "params": par, "gatew": gw,
            "constF": cf, "constB": cb, "constE8": ce}


_NC_CACHE = {}


def run(inputs, stage=0, cores=None):
    inp = {k: np.asarray(v) for k, v in inputs.items()}
    shared = _prep_shared(inp)
    x = inp["x"].astype(np.float32)
    cores = list(range(N_CORES)) if cores is None else cores
    in_maps = []
    for b in cores:
        m = dict(shared)
        m["xT"] = np.ascontiguousarray(x[b].T.reshape(8, 128, S))
        in_maps.append(m)
    import os
    if stage not in _NC_CACHE:
        _NC_CACHE[stage] = build_program(stage, int(os.environ.get('CSTOP', '0')))
    nc = _NC_CACHE[stage]
    res = run_bass_kernel_spmd(nc, in_maps, core_ids=list(range(len(cores))))
    outs = [np.asarray(r["outT"]).reshape(D, S).T for r in res.results]
    return np.ascontiguousarray(np.stack(outs, 0).astype(np.float32))


def kernel(**inputs):
    return run(inputs, 0)
```

```python
import numpy as np
from contextlib import ExitStack
import concourse.bass as bass
import concourse.mybir as mybir
from concourse.bass_utils import run_bass_kernel_spmd

F32 = mybir.dt.float32
BF16 = mybir.dt.bfloat16
AF = mybir.ActivationFunctionType
ALU = mybir.AluOpType
AX = mybir.AxisListType

ENGS = ("pe", "act", "dve", "pool", "sp")
S = 2048
D = 1024
L = 2
NPAR = 276
BIG = 30000.0
N_CORES = 8


class Res:
    __slots__ = ("name", "w", "readers", "dsem", "dcnt", "excl")

    def __init__(self, name, excl=False):
        self.name = name
        self.excl = excl
        self.w = None
        self.readers = []
        self.dsem = None
        self.dcnt = 0

    def inherit(self, *others):
        for o in others:
            if o.w is not None:
                self.readers.append(o.w)
            self.readers.extend(o.readers)
        return self


class Op:
    __slots__ = ("id", "eng", "fn", "deps", "dur", "dma", "sem", "semval", "idx", "tag")


class Prog:
    def __init__(self, nc, stack):
        self.nc = nc
        self.stack = stack
        self.ops = []
        self.sem = {e: stack.enter_context(nc.semaphore("c_" + e)) for e in ENGS}
        self.nsem = 0
        self.out_ops = {}

    def new_dsem(self):
        self.nsem += 1
        return self.stack.enter_context(self.nc.semaphore("d%d" % self.nsem))

    def _mk(self, e, fn, deps, dur):
        o = Op()
        o.id = len(self.ops)
        o.eng = e
        o.fn = fn
        o.deps = deps
        o.dur = dur
        o.dma = False
        o.sem = None
        o.semval = 0
        o.idx = 0
        o.tag = getattr(self, 'tag', '')
        self.ops.append(o)
        return o

    def op(self, e, fn, reads=(), writes=(), dur=300.0):
        if any(r.excl for r in reads):
            writes = list(writes) + [r for r in reads if r.excl and r not in writes]
            reads = [r for r in reads if not r.excl]
        deps = set()
        for r in reads:
            if r.w is not None:
                deps.add(r.w)
        for w in writes:
            if w.w is not None:
                deps.add(w.w)
            deps.update(w.readers)
        o = self._mk(e, fn, deps, dur)
        for r in reads:
            r.readers.append(o.id)
        for w in writes:
            w.w = o.id
            w.readers = []
        return o.id

    def dma(self, q, out, in_, reads=(), writes=(), nbytes=1 << 20):
        deps = set()
        for r in reads:
            if r.w is not None:
                deps.add(r.w)
        for w in writes:
            if w.w is not None:
                deps.add(w.w)
            deps.update(w.readers)
        tgt = writes[0] if writes else reads[0]
        if tgt.dsem is None:
            tgt.dsem = self.new_dsem()
        tgt.dcnt += 16
        o = self._mk(q, lambda eng: eng.dma_start(out=out, in_=in_), deps, 2000.0 + nbytes / 200.0)
        o.dma = True
        o.sem = tgt.dsem
        o.semval = tgt.dcnt
        if not writes:
            self.out_ops[id(tgt.dsem)] = o.id
        for r in reads:
            r.readers.append(o.id)
        for w in writes:
            w.w = o.id
            w.readers = []
        return o.id

    def schedule(self):
        import heapq
        ops = self.ops
        n = len(ops)
        succ = [[] for _ in range(n)]
        indeg = [0] * n
        for o in ops:
            o.deps.discard(o.id)
            indeg[o.id] = len(o.deps)
            for d in o.deps:
                succ[d].append(o.id)
        finish = [0.0] * n
        ready_t = [0.0] * n
        fut = {e: [] for e in ENGS}
        avail = {e: [] for e in ENGS}
        free = {e: 0.0 for e in ENGS}
        for o in ops:
            if indeg[o.id] == 0:
                heapq.heappush(fut[o.eng], (0.0, o.id))
        order = []
        per_eng = {e: [] for e in ENGS}
        done = 0
        while done < n:
            best = None
            for e in ENGS:
                f, a = fut[e], avail[e]
                while f and f[0][0] <= free[e]:
                    oid_ = heapq.heappop(f)[1]
                    heapq.heappush(a, (0 if ops[oid_].tag == 'norm' else 1, oid_))
                if a:
                    cand = (free[e], a[0][1], e, True)
                elif f:
                    cand = (f[0][0], f[0][1], e, False)
                else:
                    continue
                if best is None or cand[:2] < best[:2]:
                    best = cand
            st, oid, e, from_avail = best
            if from_avail:
                heapq.heappop(avail[e])
            else:
                heapq.heappop(fut[e])
            o = ops[oid]
            if o.dma:
                free[e] = st + 60.0
                finish[oid] = st + o.dur
            else:
                free[e] = st + o.dur
                finish[oid] = st + o.dur
            per_eng[e].append(oid)
            o.idx = len(per_eng[e])
            order.append(oid)
            done += 1
            for s_ in succ[oid]:
                so = ops[s_]
                lat = 0.0 if (so.eng == e and e == "pe" and not o.dma) else 120.0
                t = finish[oid] + lat
                if t > ready_t[s_]:
                    ready_t[s_] = t
                indeg[s_] -= 1
                if indeg[s_] == 0:
                    heapq.heappush(fut[so.eng], (ready_t[s_], s_))
        self.est_ns = max(finish) if finish else 0.0
        return order

    def finish(self, e="sp"):
        deps = set(self.out_ops.values())
        o = self._mk(e, None, deps, 10.0)
        return o.id

    def emit(self):
        nc = self.nc
        ops = self.ops
        order = self.schedule()
        cnt = {e: 0 for e in ENGS}
        for oid in order:
            o = ops[oid]
            if not o.dma and o.fn is not None:
                cnt[o.eng] += 1
                o.idx = cnt[o.eng]
        streams = {e: [] for e in ENGS}
        seen = {e: {f: 0 for f in ENGS} for e in ENGS}
        seen_d = {e: {} for e in ENGS}
        snaps = {}
        for oid in order:
            o = ops[oid]
            e = o.eng
            for d in sorted(o.deps):
                do = ops[d]
                if do.dma:
                    k = id(do.sem)
                    if seen_d[e].get(k, 0) >= do.semval:
                        continue
                    streams[e].append(("wait", do.sem, do.semval))
                    seen_d[e][k] = do.semval
                else:
                    f = do.eng
                    if f == e and e == "pe":
                        continue
                    if seen[e][f] >= do.idx:
                        continue
                    streams[e].append(("wait", self.sem[f], do.idx))
                    seen[e][f] = do.idx
                    sn = snaps[d]
                    for g, v in sn[0].items():
                        if g != e and seen[e][g] < v:
                            seen[e][g] = v
                    for k, v in sn[1].items():
                        if seen_d[e].get(k, 0) < v:
                            seen_d[e][k] = v
            if o.fn is None:
                continue
            if o.dma:
                streams[e].append(("op", o.fn, o.sem, 16))
            else:
                snaps[oid] = (dict(seen[e]), dict(seen_d[e]))
                streams[e].append(("op", o.fn, self.sem[e], 1))
        self.nwaits = sum(1 for e in ENGS for it in streams[e] if it[0] == "wait")

        def run(eng, items):
            for it in items:
                if it[0] == "wait":
                    eng.wait_ge(it[1], it[2])
                else:
                    it[1](eng).then_inc(it[2], it[3])

        with nc.Block() as block:
            @block.tensor
            def _(pe):
                run(pe, streams["pe"])

            @block.scalar
            def _(act):
                run(act, streams["act"])

            @block.vector
            def _(dve):
                run(dve, streams["dve"])

            @block.gpsimd
            def _(pool):
                run(pool, streams["pool"])

            @block.sync
            def _(sp):
                run(sp, streams["sp"])


class Arena:
    def __init__(self, nc, nbytes):
        self.t = nc.alloc_sbuf_tensor("arena", [128, nbytes // 2], BF16)
        self.nbytes = nbytes
        self.live = []

    def alloc(self, name, off, shape, dtype, nres=1):
        esz = 4 if dtype == F32 else 2
        n = int(np.prod(shape[1:]))
        nb = n * esz
        assert off % 32 == 0 and off + nb <= self.nbytes, (name, off, nb, self.nbytes)
        ap = self.t[0:shape[0], off // 2: (off + nb) // 2]
        if dtype != BF16:
            ap = ap.bitcast(dtype)
        if len(shape) > 2:
            names = " ".join("d%d" % i for i in range(1, len(shape)))
            kw = {"d%d" % i: shape[i] for i in range(1, len(shape))}
            ap = ap.rearrange("p (%s) -> p %s" % (names, names), **kw)
        res = [Res("%s%d" % (name, i)) for i in range(nres)]
        keep = []
        for (o0, o1, rl) in self.live:
            if o0 < off + nb and off < o1:
                for r in res:
                    r.inherit(*rl)
                if o0 < off:
                    keep.append((o0, off, rl))
                if off + nb < o1:
                    keep.append((off + nb, o1, rl))
            else:
                keep.append((o0, o1, rl))
        keep.append((off, off + nb, res))
        self.live = keep
        return ap, (res[0] if nres == 1 else res)


XT_OFF = 0
HT_OFF = 65536
YG_OFF = HT_OFF + 32800
CF_OFF = YG_OFF + 32768
NCF = 128 * 5 + 512
CB_OFF = CF_OFF + NCF * 4
NCB = 1024
CE_OFF = CB_OFF + NCB * 2
PR_OFF = CE_OFF
GW_OFF = PR_OFF + L * NPAR * 4
SCR = GW_OFF + 2 * 256 * 4
SCR_SZ = 69 * 1024
ARENA_BYTES = SCR + SCR_SZ

FFN_TILES = [(0, 410), (410, 820), (820, 1230), (1230, 1640), (1640, 2048)]
ROUNDS = [list(range(0, 8)), list(range(8, 15)), list(range(15, 22))]


class StopBuild(Exception):
    pass


def build_program(stage=0, cstop=0):
    nc = bass.Bass("TRN2", target_bir_lowering=False)
    d_x = nc.dram_tensor("xT", [8, 128, S], F32, kind="ExternalInput").ap()
    d_win = nc.dram_tensor("w_in", [L, 128, 8, 2832], F32, kind="ExternalInput").ap()
    d_wout = nc.dram_tensor("w_out", [L, 128, 8, 1024], F32, kind="ExternalInput").ap()
    d_wup = nc.dram_tensor("w_up", [L, 22, 128, 8, 256], F32, kind="ExternalInput").ap()
    d_wdn = nc.dram_tensor("w_dn", [L, 22, 128, 1024], F32, kind="ExternalInput").ap()
    d_par = nc.dram_tensor("params", [128, L, NPAR], F32, kind="ExternalInput").ap()
    d_gw = nc.dram_tensor("gatew", [17, L, 256], F32, kind="ExternalInput").ap()
    d_cf = nc.dram_tensor("constF", [128, NCF], F32, kind="ExternalInput").ap()
    d_cb = nc.dram_tensor("constB", [128, NCB], F32, kind="ExternalInput").ap()
    d_e8 = nc.dram_tensor("constE8", [8, S], F32, kind="ExternalInput").ap()
    d_out = nc.dram_tensor("outT", [8, 128, S], F32, kind="ExternalOutput").ap()

    with ExitStack() as st:
        P = Prog(nc, st)
        A = Arena(nc, ARENA_BYTES)
        PS = [st.enter_context(nc.psum_tensor("ps%d" % i, [128, 512], F32)) for i in range(8)]
        PSR = [Res("psb%d" % i, excl=True) for i in range(8)]

        def fsz(ap):
            n = 1
            for d in ap.shape[1:]:
                n *= d
            return n

        def vdur(eng, n, accel=1.0):
            if eng == "pool":
                return n * 2.3 + 100.0
            return n / (0.96 * accel) + 130.0

        def MM(out, lhsT, rhs, start, stop, reads, writes):
            mult = 4.0 if lhsT.dtype == F32 else 1.0
            P.op("pe", lambda e: e.matmul(out, lhsT, rhs, start=start, stop=stop), reads, writes,
                 dur=max(fsz(out), 64) / 2.4 * mult + 12.0)

        def TR(out, in_, ident, reads, writes):
            P.op("pe", lambda e: e.transpose(out, in_, ident), reads, writes, dur=260.0)

        def ACT(out, in_, func, reads, writes, bias=None, scale=None):
            kw = {}
            if bias is not None:
                kw["bias"] = bias
            if scale is not None:
                kw["scale"] = scale
            P.op("act", lambda e: e.activation(out=out, in_=in_, func=func, **kw), reads, writes,
                 dur=(fsz(out) + 220.0) / 1.2)

        def TT(eng, out, in0, in1, op, reads, writes):
            P.op(eng, lambda e: e.tensor_tensor(out=out, in0=in0, in1=in1, op=op), reads, writes, dur=vdur(eng, fsz(out)))

        def TS(eng, out, in0, s1, op0, reads, writes, s2=None, op1=None):
            if op1 is None:
                P.op(eng, lambda e: e.tensor_scalar(out=out, in0=in0, scalar1=s1, scalar2=None, op0=op0), reads, writes,
                     dur=vdur(eng, fsz(out)))
            else:
                P.op(eng, lambda e: e.tensor_scalar(out=out, in0=in0, scalar1=s1, scalar2=s2, op0=op0, op1=op1),
                     reads, writes, dur=vdur(eng, fsz(out)))

        def STT(eng, out, in0, scalar, in1, op0, op1, reads, writes):
            P.op(eng, lambda e: e.scalar_tensor_tensor(out=out, in0=in0, scalar=scalar, in1=in1, op0=op0, op1=op1),
                 reads, writes, dur=vdur(eng, fsz(out)))

        def CP(eng, out, in_, reads, writes):
            P.op(eng, lambda e: e.tensor_copy(out=out, in_=in_), reads, writes, dur=vdur(eng, fsz(out), 2.0))

        def RSUM(eng, out, in_, reads, writes):
            P.op(eng, lambda e: e.tensor_reduce(out=out, in_=in_, axis=AX.X, op=ALU.add), reads, writes,
                 dur=vdur(eng, fsz(in_)))

        def MSET(eng, ap, val, writes):
            P.op(eng, lambda e: e.memset(ap, val), (), writes, dur=vdur(eng, fsz(ap), 2.0))

        def DMA(q, out, in_, reads=(), writes=()):
            P.dma(q, out, in_, reads=reads, writes=writes, nbytes=out.shape[0] * fsz(out) * 4)

        def tiles_of(t0, t1):
            return list(range(t0 // 512, (t1 - 1) // 512 + 1))

        XT, XR = A.alloc("xT", XT_OFF, [128, 8, S], F32, nres=32)
        XR = [[XR[c * 4 + t] for t in range(4)] for c in range(8)]
        HT, HR = A.alloc("hT", HT_OFF, [128, 8, S + 2], BF16, nres=4)
        YG, YR = A.alloc("yg", YG_OFF, [128, 8, S], BF16, nres=32)
        YR = [[YR[c * 4 + t] for t in range(4)] for c in range(8)]
        CF, CFR = A.alloc("cF", CF_OFF, [128, NCF], F32)
        CB, CBR = A.alloc("cB", CB_OFF, [128, NCB], BF16)
        PR, PRR = A.alloc("par", PR_OFF, [128, L, NPAR], F32)
        GW, GWR = A.alloc("gw", GW_OFF, [17, L, 256], F32)

        identF = CF[:, 0:128]
        onesF = CF[:, 128:256]
        TriM = CF[:, 256:384]
        UTm = CF[:, 384:512]
        mask01 = CF[:, 512:640]
        bias_init = CF[:, 640:1152]
        identB = CB[:, 0:128]
        onesB = CB[:, 128:256]
        Wv = CB[:, 256:512].rearrange("p (a b) -> p a b", a=2)
        Cmask = CB[:, 512:1024].rearrange("p (a b) -> p a b", a=2)

        DMA("sp", CF, d_cf, writes=[CFR])
        DMA("sp", PR, d_par, writes=[PRR])
        DMA("sp", GW, d_gw, writes=[GWR])
        for tt in range(4):
            for c in range(8):
                DMA("sp", XT[:, c, tt * 512: tt * 512 + 512], d_x[c][:, tt * 512: tt * 512 + 512], writes=[XR[c][tt]])
        DMA("pool", CB, d_cb, writes=[CBR])
        MSET("dve", HT[:, :, 0:2], 0.0, [HR[0]])

        def rmsnorm(gcol, bank0, final=False):
            TZ = SCR + 50 * 1024
            sq, sqR = A.alloc("n_sq", TZ, [128, 2, 512], BF16, nres=2)
            ln, lnR = A.alloc("n_ln", TZ + 2048, [128, 512], F32)
            k = 0
            for tt in range(4):
                tok = slice(tt * 512, tt * 512 + 512)
                b = bank0 + (tt % 2)
                for c in range(8):
                    ACT(sq[:, k % 2, :], XT[:, c, tok], AF.Square, [XR[c][tt]], [sqR[k % 2]])
                    MM(PS[b][:, :], onesB, sq[:, k % 2, :], c == 0, c == 7, [sqR[k % 2], CBR], [PSR[b]])
                    k += 1
                ACT(ln, PS[b][:, :], AF.Ln, [PSR[b]], [lnR], bias=1e-6, scale=1.0 / D)
                ACT(ln, ln, AF.Exp, [lnR], [lnR], scale=-0.5)
                for c in range(8):
                    if not final:
                        STT("dve", HT[:, c, 2 + tt * 512: 2 + tt * 512 + 512], XT[:, c, tok], PR[:, 0, gcol + c: gcol + c + 1],
                            ln, ALU.mult, ALU.mult, [XR[c][tt], lnR, PRR], [HR[tt]])
                    else:
                        STT("dve", XT[:, c, tok], XT[:, c, tok], PR[:, 0, gcol + c: gcol + c + 1],
                            ln, ALU.mult, ALU.mult, [lnR, PRR], [XR[c][tt]])
                        DMA("sp", d_out[c][:, tok], XT[:, c, tok], reads=[XR[c][tt]])

        def rmsnorm_l(l, which, bank0):
            base = 0 if which == "mix" else 8
            TZ = SCR + 50 * 1024
            sq, sqR = A.alloc("n_sq", TZ, [128, 2, 512], BF16, nres=2)
            ln, lnR = A.alloc("n_ln", TZ + 2048, [128, 512], F32)
            k = 0
            for tt in range(4):
                tok = slice(tt * 512, tt * 512 + 512)
                b = bank0 + (tt % 2)
                for c in range(8):
                    ACT(sq[:, k % 2, :], XT[:, c, tok], AF.Square, [XR[c][tt]], [sqR[k % 2]])
                    MM(PS[b][:, :], onesB, sq[:, k % 2, :], c == 0, c == 7, [sqR[k % 2], CBR], [PSR[b]])
                    k += 1
                ACT(ln, PS[b][:, :], AF.Ln, [PSR[b]], [lnR], bias=1e-6, scale=1.0 / D)
                ACT(ln, ln, AF.Exp, [lnR], [lnR], scale=-0.5)
                for c in range(8):
                    STT("dve", HT[:, c, 2 + tt * 512: 2 + tt * 512 + 512], XT[:, c, tok],
                        PR[:, l, base + c: base + c + 1], ln, ALU.mult, ALU.mult,
                        [XR[c][tt], lnR, PRR], [HR[tt]])

        for l in range(L):
          try:
            par = lambda col, n=1: PR[:, l, col: col + n]
            P.tag = 'norm'
            rmsnorm_l(l, "mix", 6)
            P.tag = 'A'

            if stage == -1:
                for c in range(8):
                    for tt in range(4):
                        CP("dve", XT[:, c, tt * 512: tt * 512 + 512], HT[:, c, 2 + tt * 512: 2 + tt * 512 + 512], [HR[tt]], [XR[c][tt]])
                break
            WA, WAR = A.alloc("wA", SCR, [128, 8, 512], BF16)
            DMA("pool", WA, d_win[l][:, :, 0:512], writes=[WAR])
            hA, hAR = A.alloc("hA", SCR + 8192, [128, 2, 30 + S], BF16, nres=2)
            Dg, DgR = A.alloc("Dg", SCR + 16512, [128, 2, 31, 128], BF16)
            sig, sigR = A.alloc("sig", SCR + 32384, [128, 2, 512], F32, nres=2)
            cv, cvR = A.alloc("cv", SCR + 36480, [128, 2, 512], F32)
            csq, csqR = A.alloc("csq", SCR + 40576, [128, 2, 512], BF16)
            mean, meanR = A.alloc("mean", SCR + 44672, [128, 512], F32)
            b1, b1R = A.alloc("b1", SCR + 46720, [128, 512], F32)
            b2, b2R = A.alloc("b2", SCR + 48768, [128, 512], F32)
            for c in range(2):
                MSET("pool", hA[:, c, 0:30], 0.0, [hAR[c]])
                cw = PR[:, l, 16 + c * 31: 16 + c * 31 + 31]
                TT("dve", Dg[:, c, :, :], identB[:, None, :].broadcast_to([128, 31, 128]),
                   cw[:, :, None].broadcast_to([128, 31, 128]), ALU.mult, [CBR, PRR], [DgR])
            for tt in range(4):
                tok = slice(tt * 512, tt * 512 + 512)
                hsl = slice(2 + tt * 512, 2 + tt * 512 + 512)
                for c in range(2):
                    for kc in range(8):
                        MM(PS[c][:, :], WA[:, kc, c * 128: c * 128 + 128], HT[:, kc, hsl], kc == 0, kc == 7,
                           [WAR, HR[tt]], [PSR[c]])
                    for kc in range(8):
                        MM(PS[2 + c][:, :], WA[:, kc, 256 + c * 128: 256 + c * 128 + 128], HT[:, kc, hsl], kc == 0, kc == 7,
                           [WAR, HR[tt]], [PSR[2 + c]])
                    ACT(sig[:, c, :], PS[2 + c][:, :], AF.Sigmoid, [PSR[2 + c]], [sigR[c]])
                    TT("dve", hA[:, c, 30 + tt * 512: 30 + tt * 512 + 512], PS[c][:, :], sig[:, c, :], ALU.mult,
                       [PSR[c], sigR[c]], [hAR[c]])
                for c in range(2):
                    for i in range(31):
                        MM(PS[4 + c][:, :], Dg[:, c, i, :], hA[:, c, tt * 512 + i: tt * 512 + i + 512], i == 0, i == 30,
                           [DgR, hAR[c]], [PSR[4 + c]])
                    ACT(cv[:, c, :], PS[4 + c][:, :], AF.Identity, [PSR[4 + c], PRR], [cvR], bias=par(78 + c))
                ACT(csq, cv, AF.Square, [cvR], [csqR])
                for c in range(2):
                    MM(PS[6][:, :], onesF, cv[:, c, :], c == 0, c == 1, [cvR, CFR], [PSR[6]])
                for c in range(2):
                    MM(PS[7][:, :], onesB, csq[:, c, :], c == 0, c == 1, [csqR, CBR], [PSR[7]])
                TS("dve", mean, PS[6][:, :], 1.0 / 256, ALU.mult, [PSR[6]], [meanR])
                TT("dve", b1, mean, mean, ALU.mult, [meanR], [b1R])
                STT("dve", b2, PS[7][:, :], 1.0 / 256, b1, ALU.mult, ALU.subtract, [PSR[7], b1R], [b2R])
                ACT(b1, b2, AF.Ln, [b2R], [b1R], bias=1e-5)
                ACT(b2, b1, AF.Exp, [b1R], [b2R], scale=-0.5)
                for c in range(2):
                    TT("dve", cv[:, c, :], cv[:, c, :], mean, ALU.subtract, [cvR, meanR], [cvR])
                    TT("dve", cv[:, c, :], cv[:, c, :], b2, ALU.mult, [cvR, b2R], [cvR])
                    ACT(YG[:, c, tok], cv[:, c, :], AF.Silu, [cvR, PRR], [YR[c][tt]], bias=par(82 + c), scale=par(80 + c))

            if stage == 11:
                for c in range(8):
                    for tt in range(4):
                        CP("dve", XT[:, c, tt * 512: tt * 512 + 512], YG[:, c, tt * 512: tt * 512 + 512], [YR[c][tt]], [XR[c][tt]])
                break
            P.tag = 'C'
            WC, WCR = A.alloc("wC", SCR, [128, 8, 1552], BF16, nres=3)
            for pi, (c0, c1) in enumerate(((0, 512), (512, 1024), (1024, 1552))):
                DMA("pool", WC[:, :, c0:c1], d_win[l][:, :, 1280 + c0: 1280 + c1], writes=[WCR[pi]])
            o = SCR + 24832
            qT2, qTR2 = A.alloc("qT", o, [128, 2, 2, 512], BF16, nres=2); o += 4096
            kT2, kTR2 = A.alloc("kT", o, [128, 2, 2, 512], BF16, nres=2); o += 4096
            rs, rsR = A.alloc("rs", o, [128, 4, 512], BF16); o += 4096
            vtok2, vtokR2 = A.alloc("vtok", o, [128, 2, 4, 512], BF16, nres=8); o += 8192
            g162, g16R2 = A.alloc("g16", o, [17, 2, 512], F32, nres=2); o += 4096
            la2, laR2 = A.alloc("la", o, [128, 2, 256], F32, nres=2); o += 2048
            eGn2, eGnR2 = A.alloc("eGn", o, [128, 2, 256], F32, nres=2); o += 2048
            kd2, kdR2 = A.alloc("kd", o, [128, 2, 256], BF16, nres=2); o += 1024
            eG2, eGR2 = A.alloc("eG", o, [128, 2, 2, 128], F32, nres=2); o += 2048
            eGi2, eGiR2 = A.alloc("eGi", o, [128, 2, 2, 128], F32, nres=2); o += 2048
            qg2, qgR2 = A.alloc("qg", o, [128, 2, 2, 128], BF16, nres=2); o += 1024
            kg2, kgR2 = A.alloc("kg", o, [128, 2, 2, 128], BF16, nres=2); o += 1024
            Am, AmR = A.alloc("Am", o, [128, 4, 128], BF16); o += 1024
            Sf, SfR = A.alloc("Sf", o, [128, 2, 128], F32); o += 1024
            Sb, SbR = A.alloc("Sb", o, [128, 2, 2, 128], BF16, nres=2); o += 1024
            osq, osqR = A.alloc("osq", o, [128, 512], BF16); o += 1024
            orr, orrR = A.alloc("orr", o, [128, 512], F32); o += 2048
            assert o <= SCR + SCR_SZ, o - SCR
            MSET("dve", g162, 1.0, g16R2)
            MSET("dve", Sf, 0.0, [SfR])
            pb = 0
            for tc in range(4):
                tok = slice(tc * 512, tc * 512 + 512)
                hsl = slice(2 + tc * 512, 2 + tc * 512 + 512)
                s_ = tc % 2
                qT, qTR = qT2[:, s_, :, :], qTR2[s_]
                kT, kTR = kT2[:, s_, :, :], kTR2[s_]
                vtok, vtokR = vtok2[:, s_, :, :], vtokR2[s_ * 4: s_ * 4 + 4]
                g16, g16R = g162[:, s_, :], g16R2[s_]
                for (dst, dstR, col0, nch) in ((qT, qTR, 0, 2), (kT, kTR, 256, 2)):
                    for c in range(nch):
                        b = pb % 2; pb += 1
                        for kc in range(8):
                            MM(PS[b][:, :], WC[:, kc, col0 + c * 128: col0 + c * 128 + 128], HT[:, kc, hsl], kc == 0, kc == 7,
                               [WCR[0], HR[tc]], [PSR[b]])
                        ACT(dst[:, c, :], PS[b][:, :], AF.Copy, [PSR[b]], [dstR])
                for c in range(4):
                    b = pb % 2; pb += 1
                    for kc in range(8):
                        MM(PS[b][:, :], WC[:, kc, 1040 + c * 128: 1040 + c * 128 + 128], HT[:, kc, hsl], kc == 0, kc == 7,
                           [WCR[2], HR[tc]], [PSR[b]])
                    ACT(rs[:, c, :], PS[b][:, :], AF.Silu, [PSR[b]], [rsR])
                b = pb % 2; pb += 1
                for kc in range(8):
                    MM(PS[b][0:16, :], WC[:, kc, 1024:1040], HT[:, kc, hsl], kc == 0, kc == 7, [WCR[2], HR[tc]], [PSR[b]])
                ACT(g16[0:16, :], PS[b][0:16, :], AF.Copy, [PSR[b]], [g16R])
                for t in range(4):
                    b = pb % 2; pb += 1
                    h128 = slice(2 + tc * 512 + t * 128, 2 + tc * 512 + t * 128 + 128)
                    for kc in range(8):
                        MM(PS[b][:, :], HT[:, kc, h128], WC[:, kc, 512:1024], kc == 0, kc == 7, [WCR[1], HR[tc]], [PSR[b]])
                    ACT(vtok[:, t, :], PS[b][:, :], AF.Copy, [PSR[b]], [vtokR[t]])
                if cstop == 1:
                    raise StopBuild()
                for t in range(4):
                    gt = tc * 4 + t
                    g_ = gt % 2
                    la, laR = la2[:, g_, :], laR2[g_]
                    eGn, eGnR = eGn2[:, g_, :], eGnR2[g_]
                    kd, kdR = kd2[:, g_, :], kdR2[g_]
                    eG, eGR = eG2[:, g_, :, :], eGR2[g_]
                    eGi, eGiR = eGi2[:, g_, :, :], eGiR2[g_]
                    qg, qgR = qg2[:, g_, :, :], qgR2[g_]
                    kg, kgR = kg2[:, g_, :, :], kgR2[g_]
                    t128 = slice(t * 128, t * 128 + 128)
                    h128 = slice(2 + tc * 512 + t * 128, 2 + tc * 512 + t * 128 + 128)
                    MM(PS[2][:, 0:256], g16[0:17, t128], GW[0:17, l, :], True, True, [g16R, GWR], [PSR[2]])
                    ACT(la, PS[2][:, 0:256], AF.Exp, [PSR[2]], [laR], scale=-1.0)
                    ACT(la, la, AF.Ln, [laR], [laR], bias=1.0)
                    MM(PS[2][:, 256:512], UTm, la, True, True, [laR, CFR], [PSR[2]])
                    ACT(eGn, PS[2][:, 256:512], AF.Exp, [PSR[2]], [eGnR])
                    if cstop == 2:
                        raise StopBuild()
                    for kc in range(8):
                        MM(PS[3][:, 0:256], HT[:, kc, h128], WC[:, kc, 256:512], kc == 0, kc == 7, [WCR[0], HR[tc]], [PSR[3]])
                    TT("dve", kd, PS[3][:, 0:256], eGn, ALU.mult, [PSR[3], eGnR], [kdR])
                    if cstop == 3:
                        raise StopBuild()
                    for c in range(2):
                        MM(PS[3][:, 256 + c * 128: 256 + c * 128 + 128], la[:, c * 128: c * 128 + 128], TriM, True, True,
                           [laR, CFR], [PSR[3]])
                    gT = PS[3][:, 256:512].rearrange("p (a b) -> p a b", a=2)
                    ACT(eG, gT, AF.Exp, [PSR[3]], [eGR])
                    ACT(eGi, gT, AF.Exp, [PSR[3]], [eGiR], scale=-1.0)
                    STT("dve", qg, qT[:, :, t128], 0.125, eG, ALU.mult, ALU.mult, [qTR, eGR], [qgR])
                    TT("dve", kg, kT[:, :, t128], eGi, ALU.mult, [kTR, eGiR], [kgR])
                    if cstop == 4:
                        raise StopBuild()
                    for h in range(4):
                        hp = slice((h % 2) * 64, (h % 2) * 64 + 64)
                        ab = 4 if h % 2 == 0 else 7
                        MM(PS[ab][:, (h // 2) * 128: (h // 2) * 128 + 128], kg[hp, h // 2, :], qg[hp, h // 2, :], True, True,
                           [kgR, qgR], [PSR[ab]])
                    for par_ in range(2):
                        ab = 4 if par_ == 0 else 7
                        TT("dve", Am[:, par_:4:2, :], PS[ab][:, 0:256].rearrange("p (a b) -> p a b", a=2),
                           mask01[:, None, :].broadcast_to([128, 2, 128]), ALU.mult, [PSR[ab], CFR], [AmR])
                    if cstop == 5:
                        raise StopBuild()
                    for h in range(4):
                        hp = slice((h % 2) * 64, (h % 2) * 64 + 64)
                        MM(PS[5][hp, (h // 2) * 128: (h // 2) * 128 + 128], kd[:, h * 64: h * 64 + 64],
                           vtok[:, t, h * 128: h * 128 + 128], True, True, [kdR, vtokR[t]], [PSR[5]])
                    if cstop == 6:
                        raise StopBuild()
                    sb_cur = gt % 2
                    for h in range(4):
                        hp = slice((h % 2) * 64, (h % 2) * 64 + 64)
                        MM(PS[6][:, h * 128: h * 128 + 128], vtok[:, t, h * 128: h * 128 + 128], Am[:, h, :], True, gt == 0,
                           [vtokR[t], AmR], [PSR[6]])
                        if gt > 0:
                            MM(PS[6][:, h * 128: h * 128 + 128], Sb[hp, sb_cur, h // 2, :], qg[hp, h // 2, :], False, True,
                               [SbR[sb_cur], qgR], [PSR[6]])
                    if cstop == 7:
                        raise StopBuild()
                    for c in range(2):
                        STT("dve", Sf[:, c, :], Sf[:, c, :], eG[:, c, 127:128], PS[5][:, c * 128: c * 128 + 128],
                            ALU.mult, ALU.add, [SfR, eGR, PSR[5]], [SfR])
                    CP("dve", Sb[:, 1 - sb_cur, :, :], Sf, [SfR], [SbR[1 - sb_cur]])
                    if cstop == 8:
                        raise StopBuild()
                    ACT(osq, PS[6][:, :], AF.Square, [PSR[6]], [osqR])
                    MM(PS[7][:, :], onesB, osq, True, True, [osqR, CBR], [PSR[7]])
                    ACT(orr, PS[7][:, :], AF.Ln, [PSR[7]], [orrR], bias=1e-6, scale=1.0 / 128)
                    ACT(orr, orr, AF.Exp, [orrR], [orrR], scale=-0.5)
                    TT("dve", orr, PS[6][:, :], orr, ALU.mult, [PSR[6], orrR], [orrR])
                    for h in range(4):
                        STT("dve", YG[:, 4 + h, gt * 128: gt * 128 + 128], orr[:, h * 128: h * 128 + 128], par(86 + h),
                            rs[:, h, t128], ALU.mult, ALU.mult, [orrR, rsR, PRR], [YR[4 + h][tc]])

            if stage == 12:
                for c in range(8):
                    for tt in range(4):
                        CP("dve", XT[:, c, tt * 512: tt * 512 + 512], YG[:, c, tt * 512: tt * 512 + 512], [YR[c][tt]], [XR[c][tt]])
                break
            P.tag = 'B'
            WB, WBR = A.alloc("wB", SCR, [128, 8, 768], BF16, nres=3)
            for pi in range(3):
                DMA("pool", WB[:, :, pi * 256: pi * 256 + 256], d_win[l][:, :, 512 + pi * 256: 768 + pi * 256], writes=[WBR[pi]])
            o = SCR + 12288
            QZ, QZR = A.alloc("QZ", o, [128, 4, S], BF16, nres=16); o += 16384
            QZR = [[QZR[h * 4 + t] for t in range(4)] for h in range(4)]
            KZ, KZR = A.alloc("KZ", o, [128, 4, S], BF16, nres=16); o += 16384
            KZR = [[KZR[h * 4 + t] for t in range(4)] for h in range(4)]
            VA, VAR = A.alloc("VA", o, [128, 16, 2, 192], BF16, nres=16); o += 12288
            ball, ballR = A.alloc("ball", o, [128, 16, 2, 72], BF16, nres=16); o += 4608
            km, kmR = A.alloc("km", o, [128, 2, 8], F32, nres=4); o += 64
            kmb, kmbR = A.alloc("kmb", o, [128, 4, 8], BF16, nres=4); o += 64
            gs, gsR = A.alloc("gs", o, [128, 4, 8], F32); o += 128
            cmpb, cmpR = A.alloc("cmp", o, [128, 4, 8, 8], F32); o += 1024
            rank, rankR = A.alloc("rank", o, [128, 4, 8], F32); o += 128
            pT, pTR = A.alloc("pT", o, [128, 3, 512], BF16, nres=3); o += 3072
            msq, msqR = A.alloc("msq", o, [128, 2, 512], BF16, nres=2); o += 2048
            mrs, mrsR = A.alloc("mrs", o, [128, 512], F32); o += 2048
            assert o <= SCR + SCR_SZ, o - SCR
            MSET("dve", VA[:, :, :, 64:128], 1.0, VAR)
            MSET("dve", kmb, 0.0, kmbR)
            MSET("dve", ball, -BIG, ballR)
            for gt in range(16):
                own = gt // 2
                MSET("pool", ball[:, gt, :, own: 72: 64], 0.0, [ballR[gt]])
            for h in range(4):
                oh = slice((1 - h % 2) * 64, (1 - h % 2) * 64 + 64)
                MSET("dve", QZ[oh, h, :], 0.0, QZR[h])
                MSET("dve", KZ[oh, h, :], 0.0, KZR[h])
                r0 = (1 - h % 2) * 64
                DMA("pool", KZ[r0: r0 + 8, h, :], d_e8, writes=KZR[h])
            pb = 0
            for tt in range(4):
                hsl = slice(2 + tt * 512, 2 + tt * 512 + 512)
                tok = slice(tt * 512, tt * 512 + 512)
                for c in range(2):
                    b = pb % 2; pb += 1
                    for kc in range(8):
                        MM(PS[b][:, :], WB[:, kc, c * 128: c * 128 + 128], HT[:, kc, hsl], kc == 0, kc == 7, [WBR[0], HR[tt]], [PSR[b]])
                    CP("dve", QZ[0:64, 2 * c, tok], PS[b][0:64, :], [PSR[b]], [QZR[2 * c][tt]])
                    CP("dve", QZ[64:128, 2 * c + 1, tok], PS[b][64:128, :], [PSR[b]], [QZR[2 * c + 1][tt]])
                for c in range(2):
                    b = pb % 2; pb += 1
                    for kc in range(8):
                        MM(PS[b][:, :], WB[:, kc, 256 + c * 128: 256 + c * 128 + 128], HT[:, kc, hsl], kc == 0, kc == 7,
                           [WBR[1], HR[tt]], [PSR[b]])
                    CP("dve", KZ[0:64, 2 * c, tok], PS[b][0:64, :], [PSR[b]], [KZR[2 * c][tt]])
                    CP("dve", KZ[64:128, 2 * c + 1, tok], PS[b][64:128, :], [PSR[b]], [KZR[2 * c + 1][tt]])
                    RSUM("dve", km[:, c, 2 * tt: 2 * tt + 2], PS[b][:, :].rearrange("p (a b) -> p a b", a=2), [PSR[b]], [kmR[tt]])
                for par_ in range(2):
                    hp_ = slice(par_ * 64, par_ * 64 + 64)
                    CP("dve", kmb[hp_, par_:4:2, 2 * tt: 2 * tt + 2], km[hp_, :, 2 * tt: 2 * tt + 2], [kmR[tt]], [kmbR[tt]])
                for t in range(4):
                    gt = tt * 4 + t
                    b = pb % 2; pb += 1
                    h128 = slice(2 + gt * 128, 2 + gt * 128 + 128)
                    for kc in range(8):
                        MM(PS[b][:, 0:256], HT[:, kc, h128], WB[:, kc, 512:768], kc == 0, kc == 7, [WBR[2], HR[tt]], [PSR[b]])
                    src = PS[b][:, 0:256].rearrange("p (a w c) -> p a w c", a=2, w=2)
                    dstv = VA[:, gt, :, :].rearrange("p a (w c) -> p a w c", w=3)[:, :, 0:3:2, :]
                    CP("dve", dstv, src, [PSR[b]], [VAR[gt]])
            PSB3 = PS[7][:, :].bitcast(BF16)
            for gt in range(16):
                own = gt // 2
                tt = gt // 4
                t128 = slice(gt * 128, gt * 128 + 128)
                if own > 0:
                    for h in range(4):
                        MM(PS[4][:, h * 8: h * 8 + 8], QZ[:, h, t128], kmb[:, h, :], True, True,
                           [QZR[h][tt]] + [kmbR[i] for i in range((own - 1) // 2 + 1)], [PSR[4]])
                    g3 = PS[4][:, 0:32].rearrange("p (a b) -> p a b", a=4)
                    ACT(gs[:, :, 0:own], g3[:, :, 0:own], AF.Copy, [PSR[4]], [gsR])
                    gv = gs[:, :, 0:own]
                    TT("dve", cmpb[:, :, 0:own, 0:own], gv[:, :, None, :].broadcast_to([128, 4, own, own]),
                       gv[:, :, :, None].broadcast_to([128, 4, own, own]), ALU.is_gt, [gsR], [cmpR])
                    RSUM("dve", rank[:, :, 0:own], cmpb[:, :, 0:own, 0:own], [cmpR], [rankR])
                    for par_ in range(2):
                        c0 = 64 if par_ == 0 else 0
                        TS("dve", ball[:, gt, :, c0: c0 + own], rank[:, par_:4:2, 0:own], 2.5, ALU.is_ge,
                           [rankR], [ballR[gt]], s2=-BIG, op1=ALU.mult)
                for hc in range(2):
                    col = (hc * 4 + gt % 4) * 128
                    TR(PSB3[0:72, col: col + 128], ball[:, gt, hc, :], identB, [ballR[gt], CBR], [PSR[7]])
                if gt % 4 == 3:
                    tok = slice(tt * 512, tt * 512 + 512)
                    for hc in range(2):
                        CP("dve", QZ[0:8, 2 * hc + 1, tok], PSB3[0:8, hc * 512: hc * 512 + 512], [PSR[7]], [QZR[2 * hc + 1][tt]])
                        CP("dve", QZ[64:72, 2 * hc, tok], PSB3[64:72, hc * 512: hc * 512 + 512], [PSR[7]], [QZR[2 * hc][tt]])
            sb_i = 0
            g_i = 0
            for hc in range(2):
                for qc in range(4):
                    qtok = slice(qc * 512, qc * 512 + 512)
                    obs = (0, 1) if g_i % 2 == 0 else (5, 6)
                    g_i += 1
                    nk = 4 * qc + 4
                    for par_ in range(2):
                        h = 2 * hc + par_
                        ob = obs[par_]
                        for kt in range(nk):
                            sbk = 2 + (sb_i % 3)
                            pslot = sb_i % 3
                            sb_i += 1
                            kb = kt // 2
                            diag = kb in (2 * qc, 2 * qc + 1)
                            c0 = 256 if kb == 2 * qc + 1 else 0
                            cs = slice(c0, 512)
                            MM(PS[sbk][:, cs], KZ[:, h, kt * 128: kt * 128 + 128], QZ[:, h, qc * 512 + c0: qc * 512 + 512], True, not diag,
                               [KZR[h][kt // 4], QZR[h][qc]], [PSR[sbk]])
                            if diag:
                                qb = kb - 2 * qc
                                MM(PS[sbk][:, qb * 256: qb * 256 + 256], identB, Cmask[:, kt % 2, :], False, True,
                                   [CBR], [PSR[sbk]])
                            ACT(pT[:, pslot, cs], PS[sbk][:, cs], AF.Exp, [PSR[sbk]], [pTR[pslot]], scale=0.125)
                            MM(PS[ob][:, cs], VA[:, kt, hc, par_ * 64: par_ * 64 + 128], pT[:, pslot, cs], kt == 0, kt == nk - 1,
                               [VAR[kt], pTR[pslot]], [PSR[ob]])
                        ACT(msq[:, par_, :], PS[ob][:, :], AF.Square, [PSR[ob]], [msqR[par_]])
                    MM(PS[7][:, :], Wv[:, 0, :], msq[:, 0, :], True, False, [msqR[0], CBR], [PSR[7]])
                    MM(PS[7][:, :], Wv[:, 1, :], msq[:, 1, :], False, True, [msqR[1], CBR], [PSR[7]])
                    ACT(mrs, PS[7][:, :], AF.Ln, [PSR[7]], [mrsR])
                    ACT(mrs, mrs, AF.Exp, [mrsR], [mrsR], scale=-0.5)
                    for par_ in range(2):
                        hp = slice(par_ * 64, par_ * 64 + 64)
                        STT("dve", YG[hp, 2 + hc, qtok], PS[obs[par_]][hp, :], PR[hp, l, 84 + hc: 85 + hc], mrs[hp, :], ALU.mult, ALU.mult,
                            [PSR[obs[par_]], mrsR, PRR], [YR[2 + hc][qc]])

            if stage == 1 and l == 0:
                for c in range(8):
                    for tt in range(4):
                        CP("dve", XT[:, c, tt * 512: tt * 512 + 512], YG[:, c, tt * 512: tt * 512 + 512], [YR[c][tt]], [XR[c][tt]])
                break

            P.tag = 'O'
            WO, WOR = A.alloc("wO", SCR, [128, 8, 1024], BF16, nres=4)
            for pi in range(4):
                DMA("pool", WO[:, :, pi * 256: pi * 256 + 256], d_wout[l][:, :, pi * 256: pi * 256 + 256], writes=[WOR[pi]])
            pb = 0
            for tt in range(4):
                for dc in range(8):
                    tok = slice(tt * 512, tt * 512 + 512)
                    b = pb % 4; pb += 1
                    for kc in range(8):
                        MM(PS[b][:, :], WO[:, kc, dc * 128: dc * 128 + 128], YG[:, kc, tok], kc == 0, kc == 7,
                           [WOR[dc // 2], YR[kc][tt]], [PSR[b]])
                    TT("dve", XT[:, dc, tok], XT[:, dc, tok], PS[b][:, :], ALU.add, [PSR[b], XR[dc][tt]], [XR[dc][tt]])
            if stage == 2 and l == 0:
                break

            P.tag = 'norm'
            rmsnorm_l(l, "ffn", 6)
            P.tag = 'F'
            WU, WUR = A.alloc("wU", SCR, [128, 3, 8, 256], BF16, nres=3)
            WD, WDR = A.alloc("wD", SCR + 12288, [128, 8, 1024], BF16, nres=8)
            TM, TMR = A.alloc("ftmp", SCR + 28672, [128, 3, 5, 416], F32, nres=15)
            TMR = [[TMR[a * 5 + k] for k in range(5)] for a in range(3)]
            it = 0
            dpb = 0
            for rnd in ROUNDS:
                for si, j in enumerate(rnd):
                    us = j % 3
                    DMA("pool", WU[:, us, :, :], d_wup[l][j], writes=[WUR[us]])
                    DMA("pool", WD[:, si, :], d_wdn[l][j], writes=[WDR[si]])
                    for (t0, t1) in FFN_TILES:
                        w = t1 - t0
                        a = it % 3
                        bg = (it % 3) * 2
                        bv = bg + 1
                        it += 1
                        hres = [HR[i] for i in tiles_of(max(t0 - 2, 0), t1)]
                        for kc in range(8):
                            MM(PS[bg][:, 0:w + 2], WU[:, us, kc, 0:128], HT[:, kc, t0: t0 + w + 2], kc == 0, kc == 7,
                               [WUR[us]] + hres, [PSR[bg]])
                        for kc in range(8):
                            MM(PS[bv][:, 0:w + 2], WU[:, us, kc, 128:256], HT[:, kc, t0: t0 + w + 2], kc == 0, kc == 7,
                               [WUR[us]] + hres, [PSR[bv]])
                        fw = lambda ch, i: PR[:, l, 90 + ch * 3 + i: 90 + ch * 3 + i + 1]
                        fb = lambda ch: PR[:, l, 222 + ch: 223 + ch]
                        T = [TM[:, a, k, 0:w] for k in range(5)]
                        TRs = TMR[a]
                        ACT(T[0], PS[bg][:, 2: 2 + w], AF.Identity, [PSR[bg], PRR], [TRs[0]], bias=fb(j), scale=fw(j, 2))
                        STT("dve", T[1], PS[bg][:, 1: 1 + w], fw(j, 1), T[0], ALU.mult, ALU.add, [PSR[bg], TRs[0], PRR], [TRs[1]])
                        STT("dve", T[0], PS[bg][:, 0: w], fw(j, 0), T[1], ALU.mult, ALU.add, [PSR[bg], TRs[1], PRR], [TRs[0]])
                        ACT(T[1], T[0], AF.Silu, [TRs[0]], [TRs[1]])
                        ACT(T[2], PS[bv][:, 2: 2 + w], AF.Identity, [PSR[bv], PRR], [TRs[2]], bias=fb(22 + j), scale=fw(22 + j, 2))
                        STT("dve", T[3], PS[bv][:, 1: 1 + w], fw(22 + j, 1), T[2], ALU.mult, ALU.add,
                            [PSR[bv], TRs[2], PRR], [TRs[3]])
                        STT("dve", T[2], PS[bv][:, 0: w], fw(22 + j, 0), T[3], ALU.mult, ALU.add,
                            [PSR[bv], TRs[3], PRR], [TRs[2]])
                        TT("pool", YG[:, si, t0:t1], T[1], T[2], ALU.mult, [TRs[1], TRs[2]],
                           [YR[si][i] for i in tiles_of(t0, t1)])
                for tt in range(4):
                    tok = slice(tt * 512, tt * 512 + 512)
                    for dc in range(8):
                        b = 6 + (dpb % 2); dpb += 1
                        for si in range(len(rnd)):
                            MM(PS[b][:, :], WD[:, si, dc * 128: dc * 128 + 128], YG[:, si, tok], si == 0, si == len(rnd) - 1,
                               [WDR[si], YR[si][tt]], [PSR[b]])
                        TT("dve", XT[:, dc, tok], XT[:, dc, tok], PS[b][:, :], ALU.add, [PSR[b], XR[dc][tt]], [XR[dc][tt]])
            if stage == 3 and l == 0:
                break
          except StopBuild:
            break

        if stage == 0:
            rmsnorm(266, 6, final=True)
        else:
            for c in range(8):
                DMA("sp", d_out[c], XT[:, c, :], reads=XR[c])
        P.finish("sp")
        P.emit()
        global LAST_PROG
        LAST_PROG = P
    return nc


def _consts():
    cf = np.zeros((128, NCF), np.float32)
    j = np.arange(128)[:, None]
    i = np.arange(128)[None, :]
    cf[:, 0:128] = np.eye(128)
    cf[:, 128:256] = 1.0
    cf[:, 256:384] = np.where(j <= i, -1.0 / 16, 0.0)
    cf[:, 384:512] = np.where(j > i, -1.0 / 16, 0.0)
    cf[:, 512:640] = np.where(j <= i, 1.0, 0.0)
    bi = np.full((16, 4, 8), -BIG, np.float32)
    for gt in range(16):
        bi[gt, :, gt // 2] = 0.0
    cf[:, 640:1152] = bi.reshape(1, 512)
    cb = np.zeros((128, NCB), np.float32)
    cb[:, 0:128] = np.eye(128)
    cb[:, 128:256] = 1.0
    wv = np.zeros((128, 2, 128), np.float32)
    wv[0:64, 0, 0:64] = 1.0 / 64
    wv[64, 0, 0:64] = 1e-6
    wv[64:128, 1, 64:128] = 1.0 / 64
    wv[0, 1, 64:128] = 1e-6
    cb[:, 256:512] = wv.reshape(128, 256)
    cm = np.zeros((128, 2, 256), np.float32)
    k = np.arange(128)[:, None]
    q = np.arange(256)[None, :]
    for par in range(2):
        cm[:, par, :] = np.where(par * 128 + k > q, -BIG, 0.0)
    cb[:, 512:1024] = cm.reshape(128, 512)
    e8 = np.zeros((8, S), np.float32)
    for n in range(8):
        e8[n, n * 256:(n + 1) * 256] = 1.0
    return cf, cb, e8


def _prep_shared(inp):
    f = lambda a: np.ascontiguousarray(a, dtype=np.float32)
    w_in = f(inp["w_in"].reshape(L, 8, 128, 2832).transpose(0, 2, 1, 3))
    w_out = f(inp["w_out"].reshape(L, 8, 128, 1024).transpose(0, 2, 1, 3))
    wu = inp["ffn_w_up"].reshape(L, 8, 128, 2, 22, 128)
    w_up = f(wu.transpose(0, 4, 2, 1, 3, 5).reshape(L, 22, 128, 8, 256))
    w_dn = f(inp["ffn_w_down"].reshape(L, 22, 128, 1024))
    par = np.zeros((128, L, NPAR), np.float32)
    for l in range(L):
        par[:, l, 0:8] = inp["norm_mix_g"][l].reshape(8, 128).T
        par[:, l, 8:16] = inp["norm_ffn_g"][l].reshape(8, 128).T
        par[:, l, 16:78] = inp["conv_w"][l].T.reshape(2, 128, 31).transpose(1, 0, 2).reshape(128, 62)
        par[:, l, 78:80] = inp["conv_b"][l].reshape(2, 128).T
        par[:, l, 80:82] = inp["conv_ln_g"][l].reshape(2, 128).T
        par[:, l, 82:84] = inp["conv_ln_b"][l].reshape(2, 128).T
        par[:, l, 84:86] = inp["moba_out_g"][l].reshape(2, 128).T
        par[:, l, 86:90] = inp["gla_out_g"][l].reshape(4, 128).T
        par[:, l, 90:222] = inp["ffn_conv_w"][l].T.reshape(44, 128, 3).transpose(1, 0, 2).reshape(128, 132)
        par[:, l, 222:266] = inp["ffn_conv_b"][l].reshape(44, 128).T
        par[:, l, 266:274] = inp["final_g"].reshape(8, 128).T
    gw = np.zeros((17, L, 256), np.float32)
    for l in range(L):
        gw[0:16, l] = inp["gla_gate_w"][l]
        gw[16, l] = inp["gla_gate_b"][l]
    cf, cb, ce = _consts()
    return {"w_in": w_in, "w_out": w_out, "w_up": w_up, "w_dn": w_dn, "params": par, "gatew": gw,
            "constF": cf, "constB": cb, "constE8": ce}


_NC_CACHE = {}


def run(inputs, stage=0, cores=None):
    inp = {k: np.asarray(v) for k, v in inputs.items()}
    shared = _prep_shared(inp)
    x = inp["x"].astype(np.float32)
    cores = list(range(N_CORES)) if cores is None else cores
    in_maps = []
    for b in cores:
        m = dict(shared)
        m["xT"] = np.ascontiguousarray(x[b].T.reshape(8, 128, S))
        in_maps.append(m)
    import os
    if stage not in _NC_CACHE:
        _NC_CACHE[stage] = build_program(stage, int(os.environ.get('CSTOP', '0')))
    nc = _NC_CACHE[stage]
    res = run_bass_kernel_spmd(nc, in_maps, core_ids=list(range(len(cores))))
    outs = [np.asarray(r["outT"]).reshape(D, S).T for r in res.results]
    return np.ascontiguousarray(np.stack(outs, 0).astype(np.float32))


def kernel(**inputs):
    return run(inputs, 0)
```

```python
import numpy as np
from contextlib import ExitStack
import concourse.bass as bass
import concourse.mybir as mybir
from concourse.bass_utils import run_bass_kernel_spmd

F32 = mybir.dt.float32
BF16 = mybir.dt.bfloat16
AF = mybir.ActivationFunctionType
ALU = mybir.AluOpType
AX = mybir.AxisListType

ENGS = ("pe", "act", "dve", "pool", "sp")
S = 2048
D = 1024
L = 2
NPAR = 276
BIG = 30000.0
N_CORES = 8


class Res:
    __slots__ = ("name", "w", "readers", "dsem", "dcnt", "excl")

    def __init__(self, name, excl=False):
        self.name = name
        self.excl = excl
        self.w = None
        self.readers = []
        self.dsem = None
        self.dcnt = 0

    def inherit(self, *others):
        for o in others:
            if o.w is not None:
                self.readers.append(o.w)
            self.readers.extend(o.readers)
        return self


class Op:
    __slots__ = ("id", "eng", "fn", "deps", "dur", "dma", "sem", "semval", "idx", "tag")


class Prog:
    def __init__(self, nc, stack):
        self.nc = nc
        self.stack = stack
        self.ops = []
        self.sem = {e: stack.enter_context(nc.semaphore("c_" + e)) for e in ENGS}
        self.nsem = 0
        self.out_ops = {}

    def new_dsem(self):
        self.nsem += 1
        return self.stack.enter_context(self.nc.semaphore("d%d" % self.nsem))

    def _mk(self, e, fn, deps, dur):
        o = Op()
        o.id = len(self.ops)
        o.eng = e
        o.fn = fn
        o.deps = deps
        o.dur = dur
        o.dma = False
        o.sem = None
        o.semval = 0
        o.idx = 0
        o.tag = getattr(self, 'tag', '')
        self.ops.append(o)
        return o

    def op(self, e, fn, reads=(), writes=(), dur=300.0):
        if any(r.excl for r in reads):
            writes = list(writes) + [r for r in reads if r.excl and r not in writes]
            reads = [r for r in reads if not r.excl]
        deps = set()
        for r in reads:
            if r.w is not None:
                deps.add(r.w)
        for w in writes:
            if w.w is not None:
                deps.add(w.w)
            deps.update(w.readers)
        o = self._mk(e, fn, deps, dur)
        for r in reads:
            r.readers.append(o.id)
        for w in writes:
            w.w = o.id
            w.readers = []
        return o.id

    def dma(self, q, out, in_, reads=(), writes=(), nbytes=1 << 20):
        deps = set()
        for r in reads:
            if r.w is not None:
                deps.add(r.w)
        for w in writes:
            if w.w is not None:
                deps.add(w.w)
            deps.update(w.readers)
        tgt = writes[0] if writes else reads[0]
        if tgt.dsem is None:
            tgt.dsem = self.new_dsem()
        tgt.dcnt += 16
        o = self._mk(q, lambda eng: eng.dma_start(out=out, in_=in_), deps, 2000.0 + nbytes / 200.0)
        o.dma = True
        o.sem = tgt.dsem
        o.semval = tgt.dcnt
        if not writes:
            self.out_ops[id(tgt.dsem)] = o.id
        for r in reads:
            r.readers.append(o.id)
        for w in writes:
            w.w = o.id
            w.readers = []
        return o.id

    def schedule(self):
        import heapq
        ops = self.ops
        n = len(ops)
        succ = [[] for _ in range(n)]
        indeg = [0] * n
        for o in ops:
            o.deps.discard(o.id)
            indeg[o.id] = len(o.deps)
            for d in o.deps:
                succ[d].append(o.id)
        finish = [0.0] * n
        ready_t = [0.0] * n
        fut = {e: [] for e in ENGS}
        avail = {e: [] for e in ENGS}
        free = {e: 0.0 for e in ENGS}
        for o in ops:
            if indeg[o.id] == 0:
                heapq.heappush(fut[o.eng], (0.0, o.id))
        order = []
        per_eng = {e: [] for e in ENGS}
        done = 0
        while done < n:
            best = None
            for e in ENGS:
                f, a = fut[e], avail[e]
                while f and f[0][0] <= free[e]:
                    oid_ = heapq.heappop(f)[1]
                    heapq.heappush(a, (0 if ops[oid_].tag == 'norm' else 1, oid_))
                if a:
                    cand = (free[e], a[0][1], e, True)
                elif f:
                    cand = (f[0][0], f[0][1], e, False)
                else:
                    continue
                if best is None or cand[:2] < best[:2]:
                    best = cand
            st, oid, e, from_avail = best
            if from_avail:
                heapq.heappop(avail[e])
            else:
                heapq.heappop(fut[e])
            o = ops[oid]
            if o.dma:
                free[e] = st + 60.0
                finish[oid] = st + o.dur
            else:
                free[e] = st + o.dur
                finish[oid] = st + o.dur
            per_eng[e].append(oid)
            o.idx = len(per_eng[e])
            order.append(oid)
            done += 1
            for s_ in succ[oid]:
                so = ops[s_]
                lat = 0.0 if (so.eng == e and e == "pe" and not o.dma) else 120.0
                t = finish[oid] + lat
                if t > ready_t[s_]:
                    ready_t[s_] = t
                indeg[s_] -= 1
                if indeg[s_] == 0:
                    heapq.heappush(fut[so.eng], (ready_t[s_], s_))
        self.est_ns = max(finish) if finish else 0.0
        return order

    def finish(self, e="sp"):
        deps = set(self.out_ops.values())
        o = self._mk(e, None, deps, 10.0)
        return o.id

    def emit(self):
        nc = self.nc
        ops = self.ops
        order = self.schedule()
        cnt = {e: 0 for e in ENGS}
        for oid in order:
            o = ops[oid]
            if not o.dma and o.fn is not None:
                cnt[o.eng] += 1
                o.idx = cnt[o.eng]
        streams = {e: [] for e in ENGS}
        seen = {e: {f: 0 for f in ENGS} for e in ENGS}
        seen_d = {e: {} for e in ENGS}
        snaps = {}
        for oid in order:
            o = ops[oid]
            e = o.eng
            for d in sorted(o.deps):
                do = ops[d]
                if do.dma:
                    k = id(do.sem)
                    if seen_d[e].get(k, 0) >= do.semval:
                        continue
                    streams[e].append(("wait", do.sem, do.semval))
                    seen_d[e][k] = do.semval
                else:
                    f = do.eng
                    if f == e and e == "pe":
                        continue
                    if seen[e][f] >= do.idx:
                        continue
                    streams[e].append(("wait", self.sem[f], do.idx))
                    seen[e][f] = do.idx
                    sn = snaps[d]
                    for g, v in sn[0].items():
                        if g != e and seen[e][g] < v:
                            seen[e][g] = v
                    for k, v in sn[1].items():
                        if seen_d[e].get(k, 0) < v:
                            seen_d[e][k] = v
            if o.fn is None:
                continue
            if o.dma:
                streams[e].append(("op", o.fn, o.sem, 16))
            else:
                snaps[oid] = (dict(seen[e]), dict(seen_d[e]))
                streams[e].append(("op", o.fn, self.sem[e], 1))
        self.nwaits = sum(1 for e in ENGS for it in streams[e] if it[0] == "wait")

        def run(eng, items):
            for it in items:
                if it[0] == "wait":
                    eng.wait_ge(it[1], it[2])
                else:
                    it[1](eng).then_inc(it[2], it[3])

        with nc.Block() as block:
            @block.tensor
            def _(pe):
                run(pe, streams["pe"])

            @block.scalar
            def _(act):
                run(act, streams["act"])

            @block.vector
            def _(dve):
                run(dve, streams["dve"])

            @block.gpsimd
            def _(pool):
                run(pool, streams["pool"])

            @block.sync
            def _(sp):
                run(sp, streams["sp"])


class Arena:
    def __init__(self, nc, nbytes):
        self.t = nc.alloc_sbuf_tensor("arena", [128, nbytes // 2], BF16)
        self.nbytes = nbytes
        self.live = []

    def alloc(self, name, off, shape, dtype, nres=1):
        esz = 4 if dtype == F32 else 2
        n = int(np.prod(shape[1:]))
        nb = n * esz
        assert off % 32 == 0 and off + nb <= self.nbytes, (name, off, nb, self.nbytes)
        ap = self.t[0:shape[0], off // 2: (off + nb) // 2]
        if dtype != BF16:
            ap = ap.bitcast(dtype)
        if len(shape) > 2:
            names = " ".join("d%d" % i for i in range(1, len(shape)))
            kw = {"d%d" % i: shape[i] for i in range(1, len(shape))}
            ap = ap.rearrange("p (%s) -> p %s" % (names, names), **kw)
        res = [Res("%s%d" % (name, i)) for i in range(nres)]
        keep = []
        for (o0, o1, rl) in self.live:
            if o0 < off + nb and off < o1:
                for r in res:
                    r.inherit(*rl)
                if o0 < off:
                    keep.append((o0, off, rl))
                if off + nb < o1:
                    keep.append((off + nb, o1, rl))
            else:
                keep.append((o0, o1, rl))
        keep.append((off, off + nb, res))
        self.live = keep
        return ap, (res[0] if nres == 1 else res)


XT_OFF = 0
HT_OFF = 65536
YG_OFF = HT_OFF + 32800
CF_OFF = YG_OFF + 32768
NCF = 128 * 5 + 512
CB_OFF = CF_OFF + NCF * 4
NCB = 1024
CE_OFF = CB_OFF + NCB * 2
PR_OFF = CE_OFF
GW_OFF = PR_OFF + L * NPAR * 4
SCR = GW_OFF + 2 * 256 * 4
SCR_SZ = 69 * 1024
ARENA_BYTES = SCR + SCR_SZ

FFN_TILES = [(0, 410), (410, 820), (820, 1230), (1230, 1640), (1640, 2048)]
ROUNDS = [list(range(0, 8)), list(range(8, 15)), list(range(15, 22))]


class StopBuild(Exception):
    pass


def build_program(stage=0, cstop=0):
    nc = bass.Bass("TRN2", target_bir_lowering=False)
    d_x = nc.dram_tensor("xT", [8, 128, S], F32, kind="ExternalInput").ap()
    d_win = nc.dram_tensor("w_in", [L, 128, 8, 2832], F32, kind="ExternalInput").ap()
    d_wout = nc.dram_tensor("w_out", [L, 128, 8, 1024], F32, kind="ExternalInput").ap()
    d_wup = nc.dram_tensor("w_up", [L, 22, 128, 8, 256], F32, kind="ExternalInput").ap()
    d_wdn = nc.dram_tensor("w_dn", [L, 22, 128, 1024], F32, kind="ExternalInput").ap()
    d_par = nc.dram_tensor("params", [128, L, NPAR], F32, kind="ExternalInput").ap()
    d_gw = nc.dram_tensor("gatew", [17, L, 256], F32, kind="ExternalInput").ap()
    d_cf = nc.dram_tensor("constF", [128, NCF], F32, kind="ExternalInput").ap()
    d_cb = nc.dram_tensor("constB", [128, NCB], F32, kind="ExternalInput").ap()
    d_e8 = nc.dram_tensor("constE8", [8, S], F32, kind="ExternalInput").ap()
    d_out = nc.dram_tensor("outT", [8, 128, S], F32, kind="ExternalOutput").ap()

    with ExitStack() as st:
        P = Prog(nc, st)
        A = Arena(nc, ARENA_BYTES)
        PS = [st.enter_context(nc.psum_tensor("ps%d" % i, [128, 512], F32)) for i in range(8)]
        PSR = [Res("psb%d" % i, excl=True) for i in range(8)]

        def fsz(ap):
            n = 1
            for d in ap.shape[1:]:
                n *= d
            return n

        def vdur(eng, n, accel=1.0):
            if eng == "pool":
                return n * 2.3 + 100.0
            return n / (0.96 * accel) + 130.0

        def MM(out, lhsT, rhs, start, stop, reads, writes):
            mult = 4.0 if lhsT.dtype == F32 else 1.0
            P.op("pe", lambda e: e.matmul(out, lhsT, rhs, start=start, stop=stop), reads, writes,
                 dur=max(fsz(out), 64) / 2.4 * mult + 12.0)

        def TR(out, in_, ident, reads, writes):
            P.op("pe", lambda e: e.transpose(out, in_, ident), reads, writes, dur=260.0)

        def ACT(out, in_, func, reads, writes, bias=None, scale=None):
            kw = {}
            if bias is not None:
                kw["bias"] = bias
            if scale is not None:
                kw["scale"] = scale
            P.op("act", lambda e: e.activation(out=out, in_=in_, func=func, **kw), reads, writes,
                 dur=(fsz(out) + 220.0) / 1.2)

        def TT(eng, out, in0, in1, op, reads, writes):
            P.op(eng, lambda e: e.tensor_tensor(out=out, in0=in0, in1=in1, op=op), reads, writes, dur=vdur(eng, fsz(out)))

        def TS(eng, out, in0, s1, op0, reads, writes, s2=None, op1=None):
            if op1 is None:
                P.op(eng, lambda e: e.tensor_scalar(out=out, in0=in0, scalar1=s1, scalar2=None, op0=op0), reads, writes,
                     dur=vdur(eng, fsz(out)))
            else:
                P.op(eng, lambda e: e.tensor_scalar(out=out, in0=in0, scalar1=s1, scalar2=s2, op0=op0, op1=op1),
                     reads, writes, dur=vdur(eng, fsz(out)))

        def STT(eng, out, in0, scalar, in1, op0, op1, reads, writes):
            P.op(eng, lambda e: e.scalar_tensor_tensor(out=out, in0=in0, scalar=scalar, in1=in1, op0=op0, op1=op1),
                 reads, writes, dur=vdur(eng, fsz(out)))

        def CP(eng, out, in_, reads, writes):
            P.op(eng, lambda e: e.tensor_copy(out=out, in_=in_), reads, writes, dur=vdur(eng, fsz(out), 2.0))

        def RSUM(eng, out, in_, reads, writes):
            P.op(eng, lambda e: e.tensor_reduce(out=out, in_=in_, axis=AX.X, op=ALU.add), reads, writes,
                 dur=vdur(eng, fsz(in_)))

        def MSET(eng, ap, val, writes):
            P.op(eng, lambda e: e.memset(ap, val), (), writes, dur=vdur(eng, fsz(ap), 2.0))

        def DMA(q, out, in_, reads=(), writes=()):
            P.dma(q, out, in_, reads=reads, writes=writes, nbytes=out.shape[0] * fsz(out) * 4)

        def tiles_of(t0, t1):
            return list(range(t0 // 512, (t1 - 1) // 512 + 1))

        XT, XR = A.alloc("xT", XT_OFF, [128, 8, S], F32, nres=32)
        XR = [[XR[c * 4 + t] for t in range(4)] for c in range(8)]
        HT, HR = A.alloc("hT", HT_OFF, [128, 8, S + 2], BF16, nres=4)
        YG, YR = A.alloc("yg", YG_OFF, [128, 8, S], BF16, nres=32)
        YR = [[YR[c * 4 + t] for t in range(4)] for c in range(8)]
        CF, CFR = A.alloc("cF", CF_OFF, [128, NCF], F32)
        CB, CBR = A.alloc("cB", CB_OFF, [128, NCB], BF16)
        PR, PRR = A.alloc("par", PR_OFF, [128, L, NPAR], F32)
        GW, GWR = A.alloc("gw", GW_OFF, [17, L, 256], F32)

        identF = CF[:, 0:128]
        onesF = CF[:, 128:256]
        TriM = CF[:, 256:384]
        UTm = CF[:, 384:512]
        mask01 = CF[:, 512:640]
        bias_init = CF[:, 640:1152]
        identB = CB[:, 0:128]
        onesB = CB[:, 128:256]
        Wv = CB[:, 256:512].rearrange("p (a b) -> p a b", a=2)
        Cmask = CB[:, 512:1024].rearrange("p (a b) -> p a b", a=2)

        DMA("sp", CF, d_cf, writes=[CFR])
        DMA("sp", PR, d_par, writes=[PRR])
        DMA("sp", GW, d_gw, writes=[GWR])
        for tt in range(4):
            for c in range(8):
                DMA("sp", XT[:, c, tt * 512: tt * 512 + 512], d_x[c][:, tt * 512: tt * 512 + 512], writes=[XR[c][tt]])
        DMA("pool", CB, d_cb, writes=[CBR])
        MSET("dve", HT[:, :, 0:2], 0.0, [HR[0]])

        def rmsnorm(gcol, bank0, final=False):
            TZ = SCR + 50 * 1024
            sq, sqR = A.alloc("n_sq", TZ, [128, 2, 512], BF16, nres=2)
            ln, lnR = A.alloc("n_ln", TZ + 2048, [128, 512], F32)
            k = 0
            for tt in range(4):
                tok = slice(tt * 512, tt * 512 + 512)
                b = bank0 + (tt % 2)
                for c in range(8):
                    ACT(sq[:, k % 2, :], XT[:, c, tok], AF.Square, [XR[c][tt]], [sqR[k % 2]])
                    MM(PS[b][:, :], onesB, sq[:, k % 2, :], c == 0, c == 7, [sqR[k % 2], CBR], [PSR[b]])
                    k += 1
                ACT(ln, PS[b][:, :], AF.Ln, [PSR[b]], [lnR], bias=1e-6, scale=1.0 / D)
                ACT(ln, ln, AF.Exp, [lnR], [lnR], scale=-0.5)
                for c in range(8):
                    if not final:
                        STT("dve", HT[:, c, 2 + tt * 512: 2 + tt * 512 + 512], XT[:, c, tok], PR[:, 0, gcol + c: gcol + c + 1],
                            ln, ALU.mult, ALU.mult, [XR[c][tt], lnR, PRR], [HR[tt]])
                    else:
                        STT("dve", XT[:, c, tok], XT[:, c, tok], PR[:, 0, gcol + c: gcol + c + 1],
                            ln, ALU.mult, ALU.mult, [lnR, PRR], [XR[c][tt]])
                        DMA("sp", d_out[c][:, tok], XT[:, c, tok], reads=[XR[c][tt]])

        def rmsnorm_l(l, which, bank0):
            base = 0 if which == "mix" else 8
            TZ = SCR + 50 * 1024
            sq, sqR = A.alloc("n_sq", TZ, [128, 2, 512], BF16, nres=2)
            ln, lnR = A.alloc("n_ln", TZ + 2048, [128, 512], F32)
            k = 0
            for tt in range(4):
                tok = slice(tt * 512, tt * 512 + 512)
                b = bank0 + (tt % 2)
                for c in range(8):
                    ACT(sq[:, k % 2, :], XT[:, c, tok], AF.Square, [XR[c][tt]], [sqR[k % 2]])
                    MM(PS[b][:, :], onesB, sq[:, k % 2, :], c == 0, c == 7, [sqR[k % 2], CBR], [PSR[b]])
                    k += 1
                ACT(ln, PS[b][:, :], AF.Ln, [PSR[b]], [lnR], bias=1e-6, scale=1.0 / D)
                ACT(ln, ln, AF.Exp, [lnR], [lnR], scale=-0.5)
                for c in range(8):
                    STT("dve", HT[:, c, 2 + tt * 512: 2 + tt * 512 + 512], XT[:, c, tok],
                        PR[:, l, base + c: base + c + 1], ln, ALU.mult, ALU.mult,
                        [XR[c][tt], lnR, PRR], [HR[tt]])

        for l in range(L):
          try:
            par = lambda col, n=1: PR[:, l, col: col + n]
            P.tag = 'norm'
            rmsnorm_l(l, "mix", 6)
            P.tag = 'A'

            if stage == -1:
                for c in range(8):
                    for tt in range(4):
                        CP("dve", XT[:, c, tt * 512: tt * 512 + 512], HT[:, c, 2 + tt * 512: 2 + tt * 512 + 512], [HR[tt]], [XR[c][tt]])
                break
            WA, WAR = A.alloc("wA", SCR, [128, 8, 512], BF16)
            DMA("pool", WA, d_win[l][:, :, 0:512], writes=[WAR])
            hA, hAR = A.alloc("hA", SCR + 8192, [128, 2, 30 + S], BF16, nres=2)
            Dg, DgR = A.alloc("Dg", SCR + 16512, [128, 2, 31, 128], BF16)
            sig, sigR = A.alloc("sig", SCR + 32384, [128, 2, 512], F32, nres=2)
            cv, cvR = A.alloc("cv", SCR + 36480, [128, 2, 512], F32)
            csq, csqR = A.alloc("csq", SCR + 40576, [128, 2, 512], BF16)
            mean, meanR = A.alloc("mean", SCR + 44672, [128, 512], F32)
            b1, b1R = A.alloc("b1", SCR + 46720, [128, 512], F32)
            b2, b2R = A.alloc("b2", SCR + 48768, [128, 512], F32)
            for c in range(2):
                MSET("pool", hA[:, c, 0:30], 0.0, [hAR[c]])
                cw = PR[:, l, 16 + c * 31: 16 + c * 31 + 31]
                TT("dve", Dg[:, c, :, :], identB[:, None, :].broadcast_to([128, 31, 128]),
                   cw[:, :, None].broadcast_to([128, 31, 128]), ALU.mult, [CBR, PRR], [DgR])
            for tt in range(4):
                tok = slice(tt * 512, tt * 512 + 512)
                hsl = slice(2 + tt * 512, 2 + tt * 512 + 512)
                for c in range(2):
                    for kc in range(8):
                        MM(PS[c][:, :], WA[:, kc, c * 128: c * 128 + 128], HT[:, kc, hsl], kc == 0, kc == 7,
                           [WAR, HR[tt]], [PSR[c]])
                    for kc in range(8):
                        MM(PS[2 + c][:, :], WA[:, kc, 256 + c * 128: 256 + c * 128 + 128], HT[:, kc, hsl], kc == 0, kc == 7,
                           [WAR, HR[tt]], [PSR[2 + c]])
                    ACT(sig[:, c, :], PS[2 + c][:, :], AF.Sigmoid, [PSR[2 + c]], [sigR[c]])
                    TT("dve", hA[:, c, 30 + tt * 512: 30 + tt * 512 + 512], PS[c][:, :], sig[:, c, :], ALU.mult,
                       [PSR[c], sigR[c]], [hAR[c]])
                for c in range(2):
                    for i in range(31):
                        MM(PS[4 + c][:, :], Dg[:, c, i, :], hA[:, c, tt * 512 + i: tt * 512 + i + 512], i == 0, i == 30,
                           [DgR, hAR[c]], [PSR[4 + c]])
                    ACT(cv[:, c, :], PS[4 + c][:, :], AF.Identity, [PSR[4 + c], PRR], [cvR], bias=par(78 + c))
                ACT(csq, cv, AF.Square, [cvR], [csqR])
                for c in range(2):
                    MM(PS[6][:, :], onesF, cv[:, c, :], c == 0, c == 1, [cvR, CFR], [PSR[6]])
                for c in range(2):
                    MM(PS[7][:, :], onesB, csq[:, c, :], c == 0, c == 1, [csqR, CBR], [PSR[7]])
                TS("dve", mean, PS[6][:, :], 1.0 / 256, ALU.mult, [PSR[6]], [meanR])
                TT("dve", b1, mean, mean, ALU.mult, [meanR], [b1R])
                STT("dve", b2, PS[7][:, :], 1.0 / 256, b1, ALU.mult, ALU.subtract, [PSR[7], b1R], [b2R])
                ACT(b1, b2, AF.Ln, [b2R], [b1R], bias=1e-5)
                ACT(b2, b1, AF.Exp, [b1R], [b2R], scale=-0.5)
                for c in range(2):
                    TT("dve", cv[:, c, :], cv[:, c, :], mean, ALU.subtract, [cvR, meanR], [cvR])
                    TT("dve", cv[:, c, :], cv[:, c, :], b2, ALU.mult, [cvR, b2R], [cvR])
                    ACT(YG[:, c, tok], cv[:, c, :], AF.Silu, [cvR, PRR], [YR[c][tt]], bias=par(82 + c), scale=par(80 + c))

            if stage == 11:
                for c in range(8):
                    for tt in range(4):
                        CP("dve", XT[:, c, tt * 512: tt * 512 + 512], YG[:, c, tt * 512: tt * 512 + 512], [YR[c][tt]], [XR[c][tt]])
                break
            P.tag = 'C'
            WC, WCR = A.alloc("wC", SCR, [128, 8, 1552], BF16, nres=3)
            for pi, (c0, c1) in enumerate(((0, 512), (512, 1024), (1024, 1552))):
                DMA("pool", WC[:, :, c0:c1], d_win[l][:, :, 1280 + c0: 1280 + c1], writes=[WCR[pi]])
            o = SCR + 24832
            qT2, qTR2 = A.alloc("qT", o, [128, 2, 2, 512], BF16, nres=2); o += 4096
            kT2, kTR2 = A.alloc("kT", o, [128, 2, 2, 512], BF16, nres=2); o += 4096
            rs, rsR = A.alloc("rs", o, [128, 4, 512], BF16); o += 4096
            vtok2, vtokR2 = A.alloc("vtok", o, [128, 2, 4, 512], BF16, nres=8); o += 8192
            g162, g16R2 = A.alloc("g16", o, [17, 2, 512], F32, nres=2); o += 4096
            la2, laR2 = A.alloc("la", o, [128, 2, 256], F32, nres=2); o += 2048
            eGn2, eGnR2 = A.alloc("eGn", o, [128, 2, 256], F32, nres=2); o += 2048
            kd2, kdR2 = A.alloc("kd", o, [128, 2, 256], BF16, nres=2); o += 1024
            eG2, eGR2 = A.alloc("eG", o, [128, 2, 2, 128], F32, nres=2); o += 2048
            eGi2, eGiR2 = A.alloc("eGi", o, [128, 2, 2, 128], F32, nres=2); o += 2048
            qg2, qgR2 = A.alloc("qg", o, [128, 2, 2, 128], BF16, nres=2); o += 1024
            kg2, kgR2 = A.alloc("kg", o, [128, 2, 2, 128], BF16, nres=2); o += 1024
            Am, AmR = A.alloc("Am", o, [128, 4, 128], BF16); o += 1024
            Sf, SfR = A.alloc("Sf", o, [128, 2, 128], F32); o += 1024
            Sb, SbR = A.alloc("Sb", o, [128, 2, 2, 128], BF16, nres=2); o += 1024
            osq, osqR = A.alloc("osq", o, [128, 512], BF16); o += 1024
            orr, orrR = A.alloc("orr", o, [128, 512], F32); o += 2048
            assert o <= SCR + SCR_SZ, o - SCR
            MSET("dve", g162, 1.0, g16R2)
            MSET("dve", Sf, 0.0, [SfR])
            pb = 0
            for tc in range(4):
                tok = slice(tc * 512, tc * 512 + 512)
                hsl = slice(2 + tc * 512, 2 + tc * 512 + 512)
                s_ = tc % 2
                qT, qTR = qT2[:, s_, :, :], qTR2[s_]
                kT, kTR = kT2[:, s_, :, :], kTR2[s_]
                vtok, vtokR = vtok2[:, s_, :, :], vtokR2[s_ * 4: s_ * 4 + 4]
                g16, g16R = g162[:, s_, :], g16R2[s_]
                for (dst, dstR, col0, nch) in ((qT, qTR, 0, 2), (kT, kTR, 256, 2)):
                    for c in range(nch):
                        b = pb % 2; pb += 1
                        for kc in range(8):
                            MM(PS[b][:, :], WC[:, kc, col0 + c * 128: col0 + c * 128 + 128], HT[:, kc, hsl], kc == 0, kc == 7,
                               [WCR[0], HR[tc]], [PSR[b]])
                        ACT(dst[:, c, :], PS[b][:, :], AF.Copy, [PSR[b]], [dstR])
                for c in range(4):
                    b = pb % 2; pb += 1
                    for kc in range(8):
                        MM(PS[b][:, :], WC[:, kc, 1040 + c * 128: 1040 + c * 128 + 128], HT[:, kc, hsl], kc == 0, kc == 7,
                           [WCR[2], HR[tc]], [PSR[b]])
                    ACT(rs[:, c, :], PS[b][:, :], AF.Silu, [PSR[b]], [rsR])
                b = pb % 2; pb += 1
                for kc in range(8):
                    MM(PS[b][0:16, :], WC[:, kc, 1024:1040], HT[:, kc, hsl], kc == 0, kc == 7, [WCR[2], HR[tc]], [PSR[b]])
                ACT(g16[0:16, :], PS[b][0:16, :], AF.Copy, [PSR[b]], [g16R])
                for t in range(4):
                    b = pb % 2; pb += 1
                    h128 = slice(2 + tc * 512 + t * 128, 2 + tc * 512 + t * 128 + 128)
                    for kc in range(8):
                        MM(PS[b][:, :], HT[:, kc, h128], WC[:, kc, 512:1024], kc == 0, kc == 7, [WCR[1], HR[tc]], [PSR[b]])
                    ACT(vtok[:, t, :], PS[b][:, :], AF.Copy, [PSR[b]], [vtokR[t]])
                if cstop == 1:
                    raise StopBuild()
                for t in range(4):
                    gt = tc * 4 + t
                    g_ = gt % 2
                    la, laR = la2[:, g_, :], laR2[g_]
                    eGn, eGnR = eGn2[:, g_, :], eGnR2[g_]
                    kd, kdR = kd2[:, g_, :], kdR2[g_]
                    eG, eGR = eG2[:, g_, :, :], eGR2[g_]
                    eGi, eGiR = eGi2[:, g_, :, :], eGiR2[g_]
                    qg, qgR = qg2[:, g_, :, :], qgR2[g_]
                    kg, kgR = kg2[:, g_, :, :], kgR2[g_]
                    t128 = slice(t * 128, t * 128 + 128)
                    h128 = slice(2 + tc * 512 + t * 128, 2 + tc * 512 + t * 128 + 128)
                    MM(PS[2][:, 0:256], g16[0:17, t128], GW[0:17, l, :], True, True, [g16R, GWR], [PSR[2]])
                    ACT(la, PS[2][:, 0:256], AF.Exp, [PSR[2]], [laR], scale=-1.0)
                    ACT(la, la, AF.Ln, [laR], [laR], bias=1.0)
                    MM(PS[2][:, 256:512], UTm, la, True, True, [laR, CFR], [PSR[2]])
                    ACT(eGn, PS[2][:, 256:512], AF.Exp, [PSR[2]], [eGnR])
                    if cstop == 2:
                        raise StopBuild()
                    for kc in range(8):
                        MM(PS[3][:, 0:256], HT[:, kc, h128], WC[:, kc, 256:512], kc == 0, kc == 7, [WCR[0], HR[tc]], [PSR[3]])
                    TT("dve", kd, PS[3][:, 0:256], eGn, ALU.mult, [PSR[3], eGnR], [kdR])
                    if cstop == 3:
                        raise StopBuild()
                    for c in range(2):
                        MM(PS[3][:, 256 + c * 128: 256 + c * 128 + 128], la[:, c * 128: c * 128 + 128], TriM, True, True,
                           [laR, CFR], [PSR[3]])
                    gT = PS[3][:, 256:512].rearrange("p (a b) -> p a b", a=2)
                    ACT(eG, gT, AF.Exp, [PSR[3]], [eGR])
                    ACT(eGi, gT, AF.Exp, [PSR[3]], [eGiR], scale=-1.0)
                    STT("dve", qg, qT[:, :, t128], 0.125, eG, ALU.mult, ALU.mult, [qTR, eGR], [qgR])
                    TT("dve", kg, kT[:, :, t128], eGi, ALU.mult, [kTR, eGiR], [kgR])
                    if cstop == 4:
                        raise StopBuild()
                    for h in range(4):
                        hp = slice((h % 2) * 64, (h % 2) * 64 + 64)
                        ab = 4 if h % 2 == 0 else 7
                        MM(PS[ab][:, (h // 2) * 128: (h // 2) * 128 + 128], kg[hp, h // 2, :], qg[hp, h // 2, :], True, True,
                           [kgR, qgR], [PSR[ab]])
                    for par_ in range(2):
                        ab = 4 if par_ == 0 else 7
                        TT("dve", Am[:, par_:4:2, :], PS[ab][:, 0:256].rearrange("p (a b) -> p a b", a=2),
                           mask01[:, None, :].broadcast_to([128, 2, 128]), ALU.mult, [PSR[ab], CFR], [AmR])
                    if cstop == 5:
                        raise StopBuild()
                    for h in range(4):
                        hp = slice((h % 2) * 64, (h % 2) * 64 + 64)
                        MM(PS[5][hp, (h // 2) * 128: (h // 2) * 128 + 128], kd[:, h * 64: h * 64 + 64],
                           vtok[:, t, h * 128: h * 128 + 128], True, True, [kdR, vtokR[t]], [PSR[5]])
                    if cstop == 6:
                        raise StopBuild()
                    sb_cur = gt % 2
                    for h in range(4):
                        hp = slice((h % 2) * 64, (h % 2) * 64 + 64)
                        MM(PS[6][:, h * 128: h * 128 + 128], vtok[:, t, h * 128: h * 128 + 128], Am[:, h, :], True, gt == 0,
                           [vtokR[t], AmR], [PSR[6]])
                        if gt > 0:
                            MM(PS[6][:, h * 128: h * 128 + 128], Sb[hp, sb_cur, h // 2, :], qg[hp, h // 2, :], False, True,
                               [SbR[sb_cur], qgR], [PSR[6]])
                    if cstop == 7:
                        raise StopBuild()
                    for c in range(2):
                        STT("dve", Sf[:, c, :], Sf[:, c, :], eG[:, c, 127:128], PS[5][:, c * 128: c * 128 + 128],
                            ALU.mult, ALU.add, [SfR, eGR, PSR[5]], [SfR])
                    CP("dve", Sb[:, 1 - sb_cur, :, :], Sf, [SfR], [SbR[1 - sb_cur]])
                    if cstop == 8:
                        raise StopBuild()
                    ACT(osq, PS[6][:, :], AF.Square, [PSR[6]], [osqR])
                    MM(PS[7][:, :], onesB, osq, True, True, [osqR, CBR], [PSR[7]])
                    ACT(orr, PS[7][:, :], AF.Ln, [PSR[7]], [orrR], bias=1e-6, scale=1.0 / 128)
                    ACT(orr, orr, AF.Exp, [orrR], [orrR], scale=-0.5)
                    TT("dve", orr, PS[6][:, :], orr, ALU.mult, [PSR[6], orrR], [orrR])
                    for h in range(4):
                        STT("dve", YG[:, 4 + h, gt * 128: gt * 128 + 128], orr[:, h * 128: h * 128 + 128], par(86 + h),
                            rs[:, h, t128], ALU.mult, ALU.mult, [orrR, rsR, PRR], [YR[4 + h][tc]])

            if stage == 12:
                for c in range(8):
                    for tt in range(4):
                        CP("dve", XT[:, c, tt * 512: tt * 512 + 512], YG[:, c, tt * 512: tt * 512 + 512], [YR[c][tt]], [XR[c][tt]])
                break
            P.tag = 'B'
            WB, WBR = A.alloc("wB", SCR, [128, 8, 768], BF16, nres=3)
            for pi in range(3):
                DMA("pool", WB[:, :, pi * 256: pi * 256 + 256], d_win[l][:, :, 512 + pi * 256: 768 + pi * 256], writes=[WBR[pi]])
            o = SCR + 12288
            QZ, QZR = A.alloc("QZ", o, [128, 4, S], BF16, nres=16); o += 16384
            QZR = [[QZR[h * 4 + t] for t in range(4)] for h in range(4)]
            KZ, KZR = A.alloc("KZ", o, [128, 4, S], BF16, nres=16); o += 16384
            KZR = [[KZR[h * 4 + t] for t in range(4)] for h in range(4)]
            VA, VAR = A.alloc("VA", o, [128, 16, 2, 192], BF16, nres=16); o += 12288
            ball, ballR = A.alloc("ball", o, [128, 16, 2, 72], BF16, nres=16); o += 4608
            km, kmR = A.alloc("km", o, [128, 2, 8], F32, nres=4); o += 64
            kmb, kmbR = A.alloc("kmb", o, [128, 4, 8], BF16, nres=4); o += 64
            gs, gsR = A.alloc("gs", o, [128, 4, 8], F32); o += 128
            cmpb, cmpR = A.alloc("cmp", o, [128, 4, 8, 8], F32); o += 1024
            rank, rankR = A.alloc("rank", o, [128, 4, 8], F32); o += 128
            pT, pTR = A.alloc("pT", o, [128, 3, 512], BF16, nres=3); o += 3072
            msq, msqR = A.alloc("msq", o, [128, 2, 512], BF16, nres=2); o += 2048
            mrs, mrsR = A.alloc("mrs", o, [128, 512], F32); o += 2048
            assert o <= SCR + SCR_SZ, o - SCR
            MSET("dve", VA[:, :, :, 64:128], 1.0, VAR)
            MSET("dve", kmb, 0.0, kmbR)
            MSET("dve", ball, -BIG, ballR)
            for gt in range(16):
                own = gt // 2
                MSET("pool", ball[:, gt, :, own: 72: 64], 0.0, [ballR[gt]])
            for h in range(4):
                oh = slice((1 - h % 2) * 64, (1 - h % 2) * 64 + 64)
                MSET("dve", QZ[oh, h, :], 0.0, QZR[h])
                MSET("dve", KZ[oh, h, :], 0.0, KZR[h])
                r0 = (1 - h % 2) * 64
                DMA("pool", KZ[r0: r0 + 8, h, :], d_e8, writes=KZR[h])
            pb = 0
            for tt in range(4):
                hsl = slice(2 + tt * 512, 2 + tt * 512 + 512)
                tok = slice(tt * 512, tt * 512 + 512)
                for c in range(2):
                    b = pb % 2; pb += 1
                    for kc in range(8):
                        MM(PS[b][:, :], WB[:, kc, c * 128: c * 128 + 128], HT[:, kc, hsl], kc == 0, kc == 7, [WBR[0], HR[tt]], [PSR[b]])
                    CP("dve", QZ[0:64, 2 * c, tok], PS[b][0:64, :], [PSR[b]], [QZR[2 * c][tt]])
                    CP("dve", QZ[64:128, 2 * c + 1, tok], PS[b][64:128, :], [PSR[b]], [QZR[2 * c + 1][tt]])
                for c in range(2):
                    b = pb % 2; pb += 1
                    for kc in range(8):
                        MM(PS[b][:, :], WB[:, kc, 256 + c * 128: 256 + c * 128 + 128], HT[:, kc, hsl], kc == 0, kc == 7,
                           [WBR[1], HR[tt]], [PSR[b]])
                    CP("dve", KZ[0:64, 2 * c, tok], PS[b][0:64, :], [PSR[b]], [KZR[2 * c][tt]])
                    CP("dve", KZ[64:128, 2 * c + 1, tok], PS[b][64:128, :], [PSR[b]], [KZR[2 * c + 1][tt]])
                    RSUM("dve", km[:, c, 2 * tt: 2 * tt + 2], PS[b][:, :].rearrange("p (a b) -> p a b", a=2), [PSR[b]], [kmR[tt]])
                for par_ in range(2):
                    hp_ = slice(par_ * 64, par_ * 64 + 64)
                    CP("dve", kmb[hp_, par_:4:2, 2 * tt: 2 * tt + 2], km[hp_, :, 2 * tt: 2 * tt + 2], [kmR[tt]], [kmbR[tt]])
                for t in range(4):
                    gt = tt * 4 + t
                    b = pb % 2; pb += 1
                    h128 = slice(2 + gt * 128, 2 + gt * 128 + 128)
                    for kc in range(8):
                        MM(PS[b][:, 0:256], HT[:, kc, h128], WB[:, kc, 512:768], kc == 0, kc == 7, [WBR[2], HR[tt]], [PSR[b]])
                    src = PS[b][:, 0:256].rearrange("p (a w c) -> p a w c", a=2, w=2)
                    dstv = VA[:, gt, :, :].rearrange("p a (w c) -> p a w c", w=3)[:, :, 0:3:2, :]
                    CP("dve", dstv, src, [PSR[b]], [VAR[gt]])
            PSB3 = PS[7][:, :].bitcast(BF16)
            for gt in range(16):
                own = gt // 2
                tt = gt // 4
                t128 = slice(gt * 128, gt * 128 + 128)
                if own > 0:
                    for h in range(4):
                        MM(PS[4][:, h * 8: h * 8 + 8], QZ[:, h, t128], kmb[:, h, :], True, True,
                           [QZR[h][tt]] + [kmbR[i] for i in range((own - 1) // 2 + 1)], [PSR[4]])
                    g3 = PS[4][:, 0:32].rearrange("p (a b) -> p a b", a=4)
                    ACT(gs[:, :, 0:own], g3[:, :, 0:own], AF.Copy, [PSR[4]], [gsR])
                    gv = gs[:, :, 0:own]
                    TT("dve", cmpb[:, :, 0:own, 0:own], gv[:, :, None, :].broadcast_to([128, 4, own, own]),
                       gv[:, :, :, None].broadcast_to([128, 4, own, own]), ALU.is_gt, [gsR], [cmpR])
                    RSUM("dve", rank[:, :, 0:own], cmpb[:, :, 0:own, 0:own], [cmpR], [rankR])
                    for par_ in range(2):
                        c0 = 64 if par_ == 0 else 0
                        TS("dve", ball[:, gt, :, c0: c0 + own], rank[:, par_:4:2, 0:own], 2.5, ALU.is_ge,
                           [rankR], [ballR[gt]], s2=-BIG, op1=ALU.mult)
                for hc in range(2):
                    col = (hc * 4 + gt % 4) * 128
                    TR(PSB3[0:72, col: col + 128], ball[:, gt, hc, :], identB, [ballR[gt], CBR], [PSR[7]])
                if gt % 4 == 3:
                    tok = slice(tt * 512, tt * 512 + 512)
                    for hc in range(2):
                        CP("dve", QZ[0:8, 2 * hc + 1, tok], PSB3[0:8, hc * 512: hc * 512 + 512], [PSR[7]], [QZR[2 * hc + 1][tt]])
                        CP("dve", QZ[64:72, 2 * hc, tok], PSB3[64:72, hc * 512: hc * 512 + 512], [PSR[7]], [QZR[2 * hc][tt]])
            sb_i = 0
            g_i = 0
            for hc in range(2):
                for qc in range(4):
                    qtok = slice(qc * 512, qc * 512 + 512)
                    obs = (0, 1) if g_i % 2 == 0 else (5, 6)
                    g_i += 1
                    nk = 4 * qc + 4
                    for par_ in range(2):
                        h = 2 * hc + par_
                        ob = obs[par_]
                        for kt in range(nk):
                            sbk = 2 + (sb_i % 3)
                            pslot = sb_i % 3
                            sb_i += 1
                            kb = kt // 2
                            diag = kb in (2 * qc, 2 * qc + 1)
                            c0 = 256 if kb == 2 * qc + 1 else 0
                            cs = slice(c0, 512)
                            MM(PS[sbk][:, cs], KZ[:, h, kt * 128: kt * 128 + 128], QZ[:, h, qc * 512 + c0: qc * 512 + 512], True, not diag,
                               [KZR[h][kt // 4], QZR[h][qc]], [PSR[sbk]])
                            if diag:
                                qb = kb - 2 * qc
                                MM(PS[sbk][:, qb * 256: qb * 256 + 256], identB, Cmask[:, kt % 2, :], False, True,
                                   [CBR], [PSR[sbk]])
                            ACT(pT[:, pslot, cs], PS[sbk][:, cs], AF.Exp, [PSR[sbk]], [pTR[pslot]], scale=0.125)
                            MM(PS[ob][:, cs], VA[:, kt, hc, par_ * 64: par_ * 64 + 128], pT[:, pslot, cs], kt == 0, kt == nk - 1,
                               [VAR[kt], pTR[pslot]], [PSR[ob]])
                        ACT(msq[:, par_, :], PS[ob][:, :], AF.Square, [PSR[ob]], [msqR[par_]])
                    MM(PS[7][:, :], Wv[:, 0, :], msq[:, 0, :], True, False, [msqR[0], CBR], [PSR[7]])
                    MM(PS[7][:, :], Wv[:, 1, :], msq[:, 1, :], False, True, [msqR[1], CBR], [PSR[7]])
                    ACT(mrs, PS[7][:, :], AF.Ln, [PSR[7]], [mrsR])
                    ACT(mrs, mrs, AF.Exp, [mrsR], [mrsR], scale=-0.5)
                    for par_ in range(2):
                        hp = slice(par_ * 64, par_ * 64 + 64)
                        STT("dve", YG[hp, 2 + hc, qtok], PS[obs[par_]][hp, :], PR[hp, l, 84 + hc: 85 + hc], mrs[hp, :], ALU.mult, ALU.mult,
                            [PSR[obs[par_]], mrsR, PRR], [YR[2 + hc][qc]])

            if stage == 1 and l == 0:
                for c in range(8):
                    for tt in range(4):
                        CP("dve", XT[:, c, tt * 512: tt * 512 + 512], YG[:, c, tt * 512: tt * 512 + 512], [YR[c][tt]], [XR[c][tt]])
                break

            P.tag = 'O'
            WO, WOR = A.alloc("wO", SCR, [128, 8, 1024], BF16, nres=4)
            for pi in range(4):
                DMA("pool", WO[:, :, pi * 256: pi * 256 + 256], d_wout[l][:, :, pi * 256: pi * 256 + 256], writes=[WOR[pi]])
            pb = 0
            for tt in range(4):
                for dc in range(8):
                    tok = slice(tt * 512, tt * 512 + 512)
                    b = pb % 4; pb += 1
                    for kc in range(8):
                        MM(PS[b][:, :], WO[:, kc, dc * 128: dc * 128 + 128], YG[:, kc, tok], kc == 0, kc == 7,
                           [WOR[dc // 2], YR[kc][tt]], [PSR[b]])
                    TT("dve", XT[:, dc, tok], XT[:, dc, tok], PS[b][:, :], ALU.add, [PSR[b], XR[dc][tt]], [XR[dc][tt]])
            if stage == 2 and l == 0:
                break

            P.tag = 'norm'
            rmsnorm_l(l, "ffn", 6)
            P.tag = 'F'
            WD, WDR = A.alloc("wD", SCR + 16384, [128, 8, 1024], BF16, nres=8)
            WU, WUR = A.alloc("wU", SCR + 32768, [128, 3, 8, 256], BF16, nres=3)
            TM, TMR = A.alloc("ftmp", SCR + 45056, [128, 3, 5, 416], F32, nres=15)
            TMR = [[TMR[a * 5 + k] for k in range(5)] for a in range(3)]
            it = 0
            dpb = 0
            for rnd in ROUNDS:
                for si, j in enumerate(rnd):
                    us = j % 3
                    DMA("pool", WU[:, us, :, :], d_wup[l][j], writes=[WUR[us]])
                    DMA("pool", WD[:, si, :], d_wdn[l][j], writes=[WDR[si]])
                    for (t0, t1) in FFN_TILES:
                        w = t1 - t0
                        a = it % 3
                        bg = (it % 3) * 2
                        bv = bg + 1
                        it += 1
                        hres = [HR[i] for i in tiles_of(max(t0 - 2, 0), t1)]
                        for kc in range(8):
                            MM(PS[bg][:, 0:w + 2], WU[:, us, kc, 0:128], HT[:, kc, t0: t0 + w + 2], kc == 0, kc == 7,
                               [WUR[us]] + hres, [PSR[bg]])
                        for kc in range(8):
                            MM(PS[bv][:, 0:w + 2], WU[:, us, kc, 128:256], HT[:, kc, t0: t0 + w + 2], kc == 0, kc == 7,
                               [WUR[us]] + hres, [PSR[bv]])
                        fw = lambda ch, i: PR[:, l, 90 + ch * 3 + i: 90 + ch * 3 + i + 1]
                        fb = lambda ch: PR[:, l, 222 + ch: 223 + ch]
                        T = [TM[:, a, k, 0:w] for k in range(5)]
                        TRs = TMR[a]
                        ACT(T[0], PS[bg][:, 2: 2 + w], AF.Identity, [PSR[bg], PRR], [TRs[0]], bias=fb(j), scale=fw(j, 2))
                        STT("dve", T[1], PS[bg][:, 1: 1 + w], fw(j, 1), T[0], ALU.mult, ALU.add, [PSR[bg], TRs[0], PRR], [TRs[1]])
                        STT("dve", T[0], PS[bg][:, 0: w], fw(j, 0), T[1], ALU.mult, ALU.add, [PSR[bg], TRs[1], PRR], [TRs[0]])
                        ACT(T[1], T[0], AF.Silu, [TRs[0]], [TRs[1]])
                        ACT(T[2], PS[bv][:, 2: 2 + w], AF.Identity, [PSR[bv], PRR], [TRs[2]], bias=fb(22 + j), scale=fw(22 + j, 2))
                        STT("dve", T[3], PS[bv][:, 1: 1 + w], fw(22 + j, 1), T[2], ALU.mult, ALU.add,
                            [PSR[bv], TRs[2], PRR], [TRs[3]])
                        STT("dve", T[2], PS[bv][:, 0: w], fw(22 + j, 0), T[3], ALU.mult, ALU.add,
                            [PSR[bv], TRs[3], PRR], [TRs[2]])
                        TT("pool", YG[:, si, t0:t1], T[1], T[2], ALU.mult, [TRs[1], TRs[2]],
                           [YR[si][i] for i in tiles_of(t0, t1)])
                for tt in range(4):
                    tok = slice(tt * 512, tt * 512 + 512)
                    for dc in range(8):
                        b = 6 + (dpb % 2); dpb += 1
                        for si in range(len(rnd)):
                            MM(PS[b][:, :], WD[:, si, dc * 128: dc * 128 + 128], YG[:, si, tok], si == 0, si == len(rnd) - 1,
                               [WDR[si], YR[si][tt]], [PSR[b]])
                        TT("dve", XT[:, dc, tok], XT[:, dc, tok], PS[b][:, :], ALU.add, [PSR[b], XR[dc][tt]], [XR[dc][tt]])
            if stage == 3 and l == 0:
                break
          except StopBuild:
            break

        if stage == 0:
            rmsnorm(266, 6, final=True)
        else:
            for c in range(8):
                DMA("sp", d_out[c], XT[:, c, :], reads=XR[c])
        P.finish("sp")
        P.emit()
        global LAST_PROG
        LAST_PROG = P
    return nc


def _consts():
    cf = np.zeros((128, NCF), np.float32)
    j = np.arange(128)[:, None]
    i = np.arange(128)[None, :]
    cf[:, 0:128] = np.eye(128)
    cf[:, 128:256] = 1.0
    cf[:, 256:384] = np.where(j <= i, -1.0 / 16, 0.0)
    cf[:, 384:512] = np.where(j > i, -1.0 / 16, 0.0)
    cf[:, 512:640] = np.where(j <= i, 1.0, 0.0)
    bi = np.full((16, 4, 8), -BIG, np.float32)
    for gt in range(16):
        bi[gt, :, gt // 2] = 0.0
    cf[:, 640:1152] = bi.reshape(1, 512)
    cb = np.zeros((128, NCB), np.float32)
    cb[:, 0:128] = np.eye(128)
    cb[:, 128:256] = 1.0
    wv = np.zeros((128, 2, 128), np.float32)
    wv[0:64, 0, 0:64] = 1.0 / 64
    wv[64, 0, 0:64] = 1e-6
    wv[64:128, 1, 64:128] = 1.0 / 64
    wv[0, 1, 64:128] = 1e-6
    cb[:, 256:512] = wv.reshape(128, 256)
    cm = np.zeros((128, 2, 256), np.float32)
    k = np.arange(128)[:, None]
    q = np.arange(256)[None, :]
    for par in range(2):
        cm[:, par, :] = np.where(par * 128 + k > q, -BIG, 0.0)
    cb[:, 512:1024] = cm.reshape(128, 512)
    e8 = np.zeros((8, S), np.float32)
    for n in range(8):
        e8[n, n * 256:(n + 1) * 256] = 1.0
    return cf, cb, e8


def _prep_shared(inp):
    f = lambda a: np.ascontiguousarray(a, dtype=np.float32)
    w_in = f(inp["w_in"].reshape(L, 8, 128, 2832).transpose(0, 2, 1, 3))
    w_out = f(inp["w_out"].reshape(L, 8, 128, 1024).transpose(0, 2, 1, 3))
    wu = inp["ffn_w_up"].reshape(L, 8, 128, 2, 22, 128)
    w_up = f(wu.transpose(0, 4, 2, 1, 3, 5).reshape(L, 22, 128, 8, 256))
    w_dn = f(inp["ffn_w_down"].reshape(L, 22, 128, 1024))
    par = np.zeros((128, L, NPAR), np.float32)
    for l in range(L):
        par[:, l, 0:8] = inp["norm_mix_g"][l].reshape(8, 128).T
        par[:, l, 8:16] = inp["norm_ffn_g"][l].reshape(8, 128).T
        par[:, l, 16:78] = inp["conv_w"][l].T.reshape(2, 128, 31).transpose(1, 0, 2).reshape(128, 62)
        par[:, l, 78:80] = inp["conv_b"][l].reshape(2, 128).T
        par[:, l, 80:82] = inp["conv_ln_g"][l].reshape(2, 128).T
        par[:, l, 82:84] = inp["conv_ln_b"][l].reshape(2, 128).T
        par[:, l, 84:86] = inp["moba_out_g"][l].reshape(2, 128).T
        par[:, l, 86:90] = inp["gla_out_g"][l].reshape(4, 128).T
        par[:, l, 90:222] = inp["ffn_conv_w"][l].T.reshape(44, 128, 3).transpose(1, 0, 2).reshape(128, 132)
        par[:, l, 222:266] = inp["ffn_conv_b"][l].reshape(44, 128).T
        par[:, l, 266:274] = inp["final_g"].reshape(8, 128).T
    gw = np.zeros((17, L, 256), np.float32)
    for l in range(L):
        gw[0:16, l] = inp["gla_gate_w"][l]
        gw[16, l] = inp["gla_gate_b"][l]
    cf, cb, ce = _consts()
    return {"w_in": w_in, "w_out": w_out, "w_up": w_up, "w_dn": w_dn, "params": par, "gatew": gw,
            "constF": cf, "constB": cb, "constE8": ce}


_NC_CACHE = {}


def run(inputs, stage=0, cores=None):
    inp = {k: np.asarray(v) for k, v in inputs.items()}
    shared = _prep_shared(inp)
    x = inp["x"].astype(np.float32)
    cores = list(range(N_CORES)) if cores is None else cores
    in_maps = []
    for b in cores:
        m = dict(shared)
        m["xT"] = np.ascontiguousarray(x[b].T.reshape(8, 128, S))
        in_maps.append(m)
    import os
    if stage not in _NC_CACHE:
        _NC_CACHE[stage] = build_program(stage, int(os.environ.get('CSTOP', '0')))
    nc = _NC_CACHE[stage]
    res = run_bass_kernel_spmd(nc, in_maps, core_ids=list(range(len(cores))))
    outs = [np.asarray(r["outT"]).reshape(D, S).T for r in res.results]
    return np.ascontiguousarray(np.stack(outs, 0).astype(np.float32))


def kernel(**inputs):
    return run(inputs, 0)
```
